# Optimizing a Trainium2 kernel written in Bass

```python
import jax, jax.numpy as jnp
from jax import lax
import numpy as np

D_MODEL = 4096
BATCH = 4
SEQ = 2048
DEPTH = 1

HEAD_DIM = 128
N_FOX_HEADS = D_MODEL // HEAD_DIM // 2
N_SB_HEADS = D_MODEL // HEAD_DIM // 2
N_MIX_HEADS = N_FOX_HEADS + N_SB_HEADS
FOX_W = N_FOX_HEADS * HEAD_DIM
SB_W = N_SB_HEADS * HEAD_DIM
MIX_W = FOX_W + SB_W
N_IN = 4 * FOX_W + N_FOX_HEADS + 3 * SB_W
BLOCK_Q = 128
N_MEM = 256
XA_HEADS = 4
XA_W = XA_HEADS * HEAD_DIM
N_EXPERTS = 32
TOP_K = 4
D_EXPERT = D_MODEL // 4
SWIGLU_LIMIT = 7.0
SWIGLU_ALPHA = 1.702
LN_EPS = 1e-5
RMS_EPS = 1e-6
DEEPNORM_ALPHA = (2 * DEPTH) ** 0.25
DEEPNORM_BETA = (8 * DEPTH) ** -0.25

kernel_name = 'hybrid_fox_stickbreak_moe_block'


def layer_norm(x, g, b):
    xf = x.astype(jnp.float32)
    mu = jnp.mean(xf, axis=-1, keepdims=True)
    var = jnp.mean(jnp.square(xf - mu), axis=-1, keepdims=True)
    return ((xf - mu) * lax.rsqrt(var + LN_EPS)).astype(x.dtype) * g + b


def rms_norm(x, g):
    xf = x.astype(jnp.float32)
    y = xf * lax.rsqrt(jnp.mean(jnp.square(xf), axis=-1, keepdims=True) + RMS_EPS)
    return y.astype(x.dtype) * g


def forgetting_attention(q, k, v, c):
    S = q.shape[2]
    scale = HEAD_DIM ** -0.5
    outs = []
    for i in range(S // BLOCK_Q):
        q0 = i * BLOCK_Q
        q1 = q0 + BLOCK_Q
        s = jnp.einsum('bhqd,bhkd->bhqk', q[:, :, q0:q1], k[:, :, :q1]).astype(jnp.float32) * scale
        s = s + c[:, :, q0:q1, None] - c[:, :, None, :q1]
        qpos = q0 + jnp.arange(BLOCK_Q)
        kpos = jnp.arange(q1)
        s = jnp.where(kpos[None, :] <= qpos[:, None], s, -jnp.inf)
        p = jax.nn.softmax(s, axis=-1).astype(v.dtype)
        outs.append(jnp.einsum('bhqk,bhkd->bhqd', p, v[:, :, :q1]))
    return jnp.concatenate(outs, axis=2)


def stick_breaking_attention(q, k, v):
    S = q.shape[2]
    scale = HEAD_DIM ** -0.5
    outs = []
    for i in range(S // BLOCK_Q):
        q0 = i * BLOCK_Q
        q1 = q0 + BLOCK_Q
        z = jnp.einsum('bhqd,bhkd->bhqk', q[:, :, q0:q1], k[:, :, :q1]).astype(jnp.float32) * scale
        qpos = q0 + jnp.arange(BLOCK_Q)
        kpos = jnp.arange(q1)
        strict = kpos[None, :] < qpos[:, None]
        log_1m = jnp.where(strict, jax.nn.log_sigmoid(-z), 0.0)
        tail = lax.cumsum(log_1m, axis=3, reverse=True) - log_1m
        a = jnp.where(strict, jnp.exp(jax.nn.log_sigmoid(z) + tail), 0.0)
        outs.append(jnp.einsum('bhqk,bhkd->bhqd', a.astype(v.dtype), v[:, :, :q1]))
    return jnp.concatenate(outs, axis=2)


def hybrid_mixer(h, w_in, b_f, fox_q_norm_g, fox_k_norm_g, mix_norm_g, w_out):
    B, S, _ = h.shape
    p = h @ w_in

    def heads(lo, n):
        return p[..., lo:lo + n * HEAD_DIM].reshape(B, S, n, HEAD_DIM)

    q_f = heads(0, N_FOX_HEADS)
    k_f = heads(FOX_W, N_FOX_HEADS)
    v_f = heads(2 * FOX_W, N_FOX_HEADS)
    g_f = heads(3 * FOX_W, N_FOX_HEADS)
    f_logit = p[..., 4 * FOX_W:4 * FOX_W + N_FOX_HEADS]
    sb0 = 4 * FOX_W + N_FOX_HEADS
    q_s = heads(sb0, N_SB_HEADS)
    k_s = heads(sb0 + SB_W, N_SB_HEADS)
    v_s = heads(sb0 + 2 * SB_W, N_SB_HEADS)

    q_f = rms_norm(q_f, fox_q_norm_g)
    k_f = rms_norm(k_f, fox_k_norm_g)
    log_f = jax.nn.log_sigmoid(f_logit.astype(jnp.float32) + b_f)
    c = jnp.cumsum(log_f, axis=1).transpose(0, 2, 1)

    def bhsd(t):
        return t.transpose(0, 2, 1, 3)

    o_f = bhsd(forgetting_attention(bhsd(q_f), bhsd(k_f), bhsd(v_f), c))
    o_s = bhsd(stick_breaking_attention(bhsd(q_s), bhsd(k_s), bhsd(v_s)))
    o_f = rms_norm(o_f, mix_norm_g[:N_FOX_HEADS]) * jax.nn.sigmoid(g_f)
    o_s = rms_norm(o_s, mix_norm_g[N_FOX_HEADS:])
    o = jnp.concatenate([o_f, o_s], axis=2).reshape(B, S, MIX_W)
    return o @ w_out


def memory_cross_attention(h, mem_n, wq, wkv, wo):
    B, S, _ = h.shape
    M = mem_n.shape[1]
    q = (h @ wq).reshape(B, S, XA_HEADS, HEAD_DIM)
    kv = mem_n @ wkv
    k = kv[..., :XA_W].reshape(B, M, XA_HEADS, HEAD_DIM)
    v = kv[..., XA_W:].reshape(B, M, XA_HEADS, HEAD_DIM)
    s = jnp.einsum('bshd,bmhd->bhsm', q, k).astype(jnp.float32) * HEAD_DIM ** -0.5
    pr = jax.nn.softmax(s, axis=-1).astype(v.dtype)
    o = jnp.einsum('bhsm,bmhd->bshd', pr, v).reshape(B, S, XA_W)
    return o @ wo


def clamped_swiglu_moe(h, router_w, router_b, w_up, b_up, w_down, b_down):
    B, S, D = h.shape
    T = B * S
    ht = h.reshape(T, D)
    logits = (ht @ router_w + router_b).astype(jnp.float32)
    top_v, top_i = lax.top_k(logits, TOP_K)
    top_w = jax.nn.softmax(top_v, axis=-1)
    gate = jnp.sum(jax.nn.one_hot(top_i, N_EXPERTS, dtype=jnp.float32) * top_w[..., None],
                   axis=1).astype(h.dtype)
    out = jnp.zeros_like(ht)
    for e in range(N_EXPERTS):
        gu = ht @ w_up[e] + b_up[e]
        g = jnp.minimum(gu[:, :D_EXPERT], SWIGLU_LIMIT)
        u = jnp.clip(gu[:, D_EXPERT:], -SWIGLU_LIMIT, SWIGLU_LIMIT)
        a = (u + 1.0) * g * jax.nn.sigmoid(SWIGLU_ALPHA * g)
        out = out + gate[:, e:e + 1] * (a @ w_down[e] + b_down[e])
    return out.reshape(B, S, D)


def setup_inputs(seed: int = 0) -> dict:
    key = jax.random.key(seed)
    keys = jax.random.split(key, 48)
    counter = iter(range(48))
    L = DEPTH
    beta = DEEPNORM_BETA
    s_d = D_MODEL ** -0.5

    def normal(shape, scale):
        return jax.random.normal(keys[next(counter)], shape, jnp.float32) * scale

    def gain(shape):
        return 1.0 + normal(shape, 0.02)

    x = normal((BATCH, SEQ, D_MODEL), 1.0)
    mem = normal((BATCH, N_MEM, D_MODEL), 1.0)
    ln_in_g = gain((D_MODEL,))
    ln_in_b = normal((D_MODEL,), 0.02)
    w_in = jnp.concatenate([
        normal((L, D_MODEL, 2 * FOX_W), s_d),
        normal((L, D_MODEL, FOX_W), s_d * beta),
        normal((L, D_MODEL, FOX_W), s_d),
        normal((L, D_MODEL, N_FOX_HEADS), s_d),
        normal((L, D_MODEL, 2 * SB_W), s_d),
        normal((L, D_MODEL, SB_W), s_d * beta),
    ], axis=-1)
    b_f = jnp.linspace(1.0, 6.0, N_FOX_HEADS, dtype=jnp.float32)[None, :] + normal((L, N_FOX_HEADS), 0.1)
    fox_q_norm_g = gain((L, HEAD_DIM))
    fox_k_norm_g = gain((L, HEAD_DIM))
    mix_norm_g = gain((L, N_MIX_HEADS, HEAD_DIM))
    w_out = normal((L, MIX_W, D_MODEL), MIX_W ** -0.5 * beta)
    ln_mix_g = gain((L, D_MODEL))
    ln_mix_b = normal((L, D_MODEL), 0.02)
    mem_ln_g = gain((L, D_MODEL))
    mem_ln_b = normal((L, D_MODEL), 0.02)
    xa_wq = normal((L, D_MODEL, XA_W), s_d)
    xa_wkv = jnp.concatenate([normal((L, D_MODEL, XA_W), s_d),
                              normal((L, D_MODEL, XA_W), s_d * beta)], axis=-1)
    xa_wo = normal((L, XA_W, D_MODEL), XA_W ** -0.5 * beta)
    ln_xa_g = gain((L, D_MODEL))
    ln_xa_b = normal((L, D_MODEL), 0.02)
    router_w = normal((L, D_MODEL, N_EXPERTS), s_d)
    router_b = normal((L, N_EXPERTS), 0.01)
    w_up = normal((L, N_EXPERTS, D_MODEL, 2 * D_EXPERT), s_d)
    b_up = normal((L, N_EXPERTS, 2 * D_EXPERT), 0.01)
    w_down = normal((L, N_EXPERTS, D_EXPERT, D_MODEL), D_EXPERT ** -0.5 * beta)
    b_down = normal((L, N_EXPERTS, D_MODEL), 0.01)
    ln_moe_g = gain((L, D_MODEL))
    ln_moe_b = normal((L, D_MODEL), 0.02)
    return {'x': x, 'mem': mem, 'ln_in_g': ln_in_g, 'ln_in_b': ln_in_b,
            'w_in': w_in, 'b_f': b_f, 'fox_q_norm_g': fox_q_norm_g, 'fox_k_norm_g': fox_k_norm_g,
            'mix_norm_g': mix_norm_g, 'w_out': w_out, 'ln_mix_g': ln_mix_g, 'ln_mix_b': ln_mix_b,
            'mem_ln_g': mem_ln_g, 'mem_ln_b': mem_ln_b, 'xa_wq': xa_wq, 'xa_wkv': xa_wkv,
            'xa_wo': xa_wo, 'ln_xa_g': ln_xa_g, 'ln_xa_b': ln_xa_b,
            'router_w': router_w, 'router_b': router_b, 'w_up': w_up, 'b_up': b_up,
            'w_down': w_down, 'b_down': b_down, 'ln_moe_g': ln_moe_g, 'ln_moe_b': ln_moe_b}


def reference(x, mem, ln_in_g, ln_in_b, w_in, b_f, fox_q_norm_g, fox_k_norm_g, mix_norm_g,
              w_out, ln_mix_g, ln_mix_b, mem_ln_g, mem_ln_b, xa_wq, xa_wkv, xa_wo,
              ln_xa_g, ln_xa_b, router_w, router_b, w_up, b_up, w_down, b_down,
              ln_moe_g, ln_moe_b):
    h = layer_norm(x, ln_in_g, ln_in_b)
    for l in range(DEPTH):
        mix = hybrid_mixer(h, w_in[l], b_f[l], fox_q_norm_g[l], fox_k_norm_g[l], mix_norm_g[l], w_out[l])
        h = layer_norm(DEEPNORM_ALPHA * h + mix, ln_mix_g[l], ln_mix_b[l])
        mem_n = layer_norm(mem, mem_ln_g[l], mem_ln_b[l])
        xa = memory_cross_attention(h, mem_n, xa_wq[l], xa_wkv[l], xa_wo[l])
        h = layer_norm(DEEPNORM_ALPHA * h + xa, ln_xa_g[l], ln_xa_b[l])
        ff = clamped_swiglu_moe(h, router_w[l], router_b[l], w_up[l], b_up[l], w_down[l], b_down[l])
        h = layer_norm(DEEPNORM_ALPHA * h + ff, ln_moe_g[l], ln_moe_b[l])
    return h
```

```python
import numpy as np
import ml_dtypes
from contextlib import ExitStack, contextmanager
import concourse.bass as bass
import concourse.mybir as mybir
from concourse.bass_utils import run_bass_kernel_spmd

F32 = mybir.dt.float32
BF16 = mybir.dt.bfloat16
I32 = mybir.dt.int32
U32 = mybir.dt.uint32
AF = mybir.ActivationFunctionType
ALU = mybir.AluOpType
AX = mybir.AxisListType

D = 4096
KC = 32
S = 2048
T = 1024
NB = 16
NQ = 8
HD = 128
NFH = 16
NSH = 16
N_IN = 14352
QF, KF, VF, GF, FL, QS, KS, VS = 0, 2048, 4096, 6144, 8192, 8208, 10256, 12304
NMEM = 256
XAW = 512
NE = 32
FE = 1024
SCALE = 128 ** -0.5
ALPHA = 2 ** 0.25
LN_EPS = 1e-5
RMS_EPS = 1e-6
NEG = -30000.0
N_CORES = 8
ENG = ("pe", "act", "dve", "pool", "sp")


class Region:
    __slots__ = ("w", "r", "excl")

    def __init__(self, excl=False):
        self.w = None
        self.r = {}
        self.excl = excl


class Prog:
    def __init__(self, nc, es, n_dma_sems=40):
        self.nc = nc
        self.q = {e: [] for e in ENG}
        self.esem = {e: es.enter_context(nc.semaphore("es_" + e)) for e in ENG}
        self.ecnt = {e: 0 for e in ENG}
        self.waited = {e: {} for e in ENG}
        self.dsems = [es.enter_context(nc.semaphore("ds%d" % i)) for i in range(n_dma_sems)]
        self.dval = [0] * n_dma_sems
        self.dnext = 0
        self.nops = 0

    def _wait(self, eng, ticket):
        sem, val = ticket
        k = id(sem)
        if self.waited[eng].get(k, 0) >= val:
            return
        self.waited[eng][k] = val
        self.q[eng].append(lambda e, s=sem, v=val: e.wait_ge(s, v))

    def op(self, eng, fn, reads=(), writes=(), dma=False):
        deps = []
        for R in reads:
            if R.w is not None:
                deps.append(R.w)
            if R.excl:
                deps.extend(R.r.values())
        for R in writes:
            if R.w is not None:
                deps.append(R.w)
            deps.extend(R.r.values())
        pes = self.esem["pe"]
        for t in deps:
            if eng == "pe" and t[0] is pes:
                continue
            self._wait(eng, t)
        if dma:
            i = self.dnext
            self.dnext = (i + 1) % len(self.dsems)
            sem = self.dsems[i]
            if self.dval[i] > 0:
                self._wait(eng, (sem, self.dval[i]))
            self.dval[i] += 16
            ticket = (sem, self.dval[i])
            self.q[eng].append(lambda e, s=sem, f=fn: f(e).then_inc(s, 16))
        else:
            self.ecnt[eng] += 1
            sem = self.esem[eng]
            ticket = (sem, self.ecnt[eng])
            self.q[eng].append(lambda e, s=sem, f=fn: f(e).then_inc(s, 1))
        for R in reads:
            R.r[id(ticket[0])] = ticket
        for R in writes:
            R.w = ticket
            R.r = {}
        self.nops += 1
        return ticket

    def fence(self):
        tickets = [(self.esem[e], self.ecnt[e]) for e in ENG if self.ecnt[e] > 0]
        tickets += [(self.dsems[i], self.dval[i]) for i in range(len(self.dsems)) if self.dval[i] > 0]
        for e in ENG:
            for t in tickets:
                if t[0] is self.esem[e]:
                    continue
                self._wait(e, t)

    def wait_all(self, eng, regions):
        for R in regions:
            if R.w is not None:
                self._wait(eng, R.w)

    def emit(self):
        nc = self.nc
        allsems = list(self.esem.values()) + list(self.dsems)
        with nc.Block() as b0:
            @b0.gpsimd
            def _(e):
                for s in allsems:
                    e.sem_clear(s)
        with nc.Block() as block:
            @block.tensor
            def _(e):
                for f in self.q["pe"]:
                    f(e)

            @block.scalar
            def _(e):
                for f in self.q["act"]:
                    f(e)

            @block.vector
            def _(e):
                for f in self.q["dve"]:
                    f(e)

            @block.gpsimd
            def _(e):
                for f in self.q["pool"]:
                    f(e)

            @block.sync
            def _(e):
                for f in self.q["sp"]:
                    f(e)
        with nc.Block() as b2:
            @b2.gpsimd
            def _(e):
                for s in allsems:
                    e.sem_clear(s)


class Tn:
    def __init__(self, t, nreg=1):
        self.t = t
        self.regs = [Region() for _ in range(nreg)]

    @property
    def R(self):
        return self.regs[0]


def _ceil_chunks(c0, c1, step):
    out = []
    while c0 < c1:
        out.append((c0, min(c0 + step, c1)))
        c0 += step
    return out


class Builder:
    def __init__(self, debug=None):
        self.debug = debug or {}
        self.nc = bass.Bass("TRN2", target_bir_lowering=False)
        self.es = ExitStack()
        self.P = Prog(self.nc, self.es)
        self.inputs = {}
        self.psum_rr = 0

    def din(self, name, shape, dt=F32):
        t = self.nc.dram_tensor(name, list(shape), dt, kind="ExternalInput")
        self.inputs[name] = t
        return t

    def dscr(self, name, shape, dt, nreg=1):
        kind = "ExternalOutput" if name in self.debug.get("outs", ()) else "Internal"
        t = self.nc.dram_tensor(name, list(shape), dt, kind=kind)
        return Tn(t, nreg)

    def dbg_t(self, name, shape, dt):
        if not hasattr(self, "_dbg"):
            self._dbg = {}
        if name not in self._dbg:
            self._dbg[name] = self.nc.dram_tensor(name, list(shape), dt, kind="ExternalOutput")
        return self._dbg[name]

    def sb(self, es, name, shape, dt, nreg=1):
        self._uid = getattr(self, "_uid", 0) + 1
        t = es.enter_context(self.nc.sbuf_tensor("%s_%d" % (name, self._uid), list(shape), dt))
        return Tn(t, nreg)

    @contextmanager
    def scope(self):
        with ExitStack() as es_:
            yield es_
            self.P.fence()

    def load(self, eng, dst_ap, src_ap, reads, writes, **kw):
        return self.P.op(eng, lambda e: e.dma_start(out=dst_ap, in_=src_ap, **kw), reads=reads, writes=writes, dma=True)

    def next_bank(self, pool):
        i = self.psum_rr % len(pool)
        self.psum_rr += 1
        return pool[i]

    def build(self):
        nc, P, es = self.nc, self.P, self.es
        dbg = self.debug
        phases = dbg.get("phases", "ABCDEF")

        x_seq = self.din("x_seq", [S, D])
        x_own = self.din("x_own", [T, D])
        mem_in = self.din("mem", [NMEM, D])
        ln_in_g = self.din("ln_in_g", [D]); ln_in_b = self.din("ln_in_b", [D])
        w_in = self.din("w_in", [D, N_IN])
        b_f = self.din("b_f", [NFH])
        fq_g = self.din("fox_q_norm_g", [HD]); fk_g = self.din("fox_k_norm_g", [HD])
        mix_g = self.din("mix_norm_g", [32, HD])
        w_out = self.din("w_out", [D, D])
        ln_mix_g = self.din("ln_mix_g", [D]); ln_mix_b = self.din("ln_mix_b", [D])
        mem_ln_g = self.din("mem_ln_g", [D]); mem_ln_b = self.din("mem_ln_b", [D])
        xa_wq = self.din("xa_wq", [D, XAW]); xa_wkv = self.din("xa_wkv", [D, 2 * XAW]); xa_wo = self.din("xa_wo", [XAW, D])
        ln_xa_g = self.din("ln_xa_g", [D]); ln_xa_b = self.din("ln_xa_b", [D])
        router_w = self.din("router_w", [D, NE]); router_b = self.din("router_b", [NE])
        NEX = dbg.get("ne", NE)
        w_up = self.din("w_up", [NEX, D, 2 * FE]); b_up = self.din("b_up", [NE, 2 * FE])
        w_down = self.din("w_down", [NEX, FE, D]); b_down = self.din("b_down", [NE, D])
        ln_moe_g = self.din("ln_moe_g", [D]); ln_moe_b = self.din("ln_moe_b", [D])
        c_ident_f = self.din("c_ident_f", [128, 128])
        c_ident_b = self.din("c_ident_b", [128, 128], BF16)
        c_ones_b = self.din("c_ones_b", [128, 128], BF16)
        c_ones_f = self.din("c_ones_f", [128, 128])
        c_tri_f = self.din("c_tri_f", [128, 128])
        c_uincl_f = self.din("c_uincl_f", [128, 128])
        c_sel96 = self.din("c_sel96", [96, NFH * 128], BF16)
        c_jones = self.din("c_jones", [128, 128])
        c_mbn = self.din("c_mbn", [128, 3, 128], BF16)
        c_mbp = self.din("c_mbp", [128, 3, 128], BF16)
        c_m01 = self.din("c_m01", [128, 3, 128])
        out_t = self.nc.dram_tensor("out", [T, D], F32, kind="ExternalOutput")
        out_regs = [Region() for _ in range(NQ)]

        kfT = self.dscr("kfT", [NFH, 128, S], BF16, NFH)
        ksT = self.dscr("ksT", [NSH, 128, S], BF16, NSH)
        vfS = self.dscr("vfS", [S, NFH * HD], BF16, 8)
        vsS = self.dscr("vsS", [S, NSH * HD], BF16, 8)
        qfT = self.dscr("qfT", [NFH, 128, T], BF16, NFH)
        qsT = self.dscr("qsT", [NSH, 128, T], BF16, NSH)
        gfT = self.dscr("gfT", [NFH, 128, T], F32, NFH)
        hown = self.dscr("hown", [T, D], F32, NQ)
        oT = self.dscr("oT", [32, 128, T], BF16, 32)
        y1 = self.dscr("y1", [T, D], F32, NQ)
        h2s = self.dscr("h2s", [T, D], F32, NQ)
        self.h2chk = self.dscr("h2chk", [T, D], F32, NQ)
        self.h2fin = self.dscr("h2fin", [T, D], F32, NQ)

        pes = ExitStack()
        self.es.enter_context(pes)
        ident_f = self.sb(pes, "ident_f", [128, 128], F32)
        ident_b = self.sb(pes, "ident_b", [128, 128], BF16)
        ones_b = self.sb(pes, "ones_b", [128, 128], BF16)
        ones_f = self.sb(pes, "ones_f", [128, 128], F32)
        tri_f = self.sb(pes, "tri_f", [128, 128], F32)
        uincl_f = self.sb(pes, "uincl_f", [128, 128], F32)
        jones = self.sb(pes, "jones", [128, 128], F32)
        sel96 = self.sb(pes, "sel96", [96, NFH * 128], BF16)
        mbn = self.sb(pes, "mbn", [128, 3, 128], BF16)
        mbp = self.sb(pes, "mbp", [128, 3, 128], BF16)
        m01 = self.sb(pes, "m01", [128, 3, 128], F32)
        negc = self.sb(pes, "negc", [128, NB, NFH], F32)
        lfseq = self.sb(pes, "lfseq", [128, NB, NFH], F32)
        cT3 = self.sb(pes, "cT3", [96, T], BF16)
        psum = [Tn(pes.enter_context(nc.psum_tensor("ps%d" % i, [128, 512], F32))) for i in range(8)]
        for b_ in psum:
            b_.regs[0].excl = True
        self.psum = psum
        for dst, src in ((ident_f, c_ident_f), (ident_b, c_ident_b), (ones_b, c_ones_b), (ones_f, c_ones_f),
                         (tri_f, c_tri_f), (uincl_f, c_uincl_f), (jones, c_jones), (sel96, c_sel96),
                         (mbn, c_mbn), (mbp, c_mbp), (m01, c_m01)):
            self.load("sp", dst.t[:], src.ap(), [], [dst.R])
        self.consts = dict(ident_f=ident_f, ident_b=ident_b, ones_b=ones_b, ones_f=ones_f, tri_f=tri_f,
                           uincl_f=uincl_f, jones=jones, sel96=sel96, mbn=mbn, mbp=mbp, m01=m01)

        self._eps = {}
        for v in (LN_EPS, RMS_EPS, 1.0):
            t = self.sb(pes, "eps%g" % v, [128, 1], F32)
            P.op("dve", lambda e, t=t, v=v: e.memset(t.t[:], v), writes=[t.R])
            self._eps["eps%g" % v] = t
        w_in_v = w_in.ap().rearrange("(k p) c -> p k c", p=128)

        def featvec(es_, name, src, n=KC):
            t = self.sb(es_, name, [128, n], F32)
            tmp = self.sb(es_, name + "_row", [n, 128], F32)
            ap = src.ap()
            if len(ap.shape) == 1:
                ap = ap.rearrange("(k p) -> k p", p=128)
            self.load("sp", tmp.t[:], ap, [], [tmp.R])
            bank = psum[7]
            P.op("pe", lambda e: e.matmul(bank.t[:, 0:n], lhsT=tmp.t[:], rhs=ident_f.t[0:n, 0:n], start=True, stop=True),
                 reads=[tmp.R, ident_f.R], writes=[bank.R])
            P.op("dve", lambda e: e.tensor_copy(out=t.t[:], in_=bank.t[:, 0:n]), reads=[bank.R], writes=[t.R])
            return t
        self.featvec = featvec

        if "A" in phases:
            self.phase_inproj(x_seq, x_own, ln_in_g, ln_in_b, w_in_v, b_f, fq_g, fk_g,
                              kfT, ksT, vfS, vsS, qfT, qsT, gfT, hown, negc, lfseq, cT3, featvec)
        if "C" in phases:
            self.phase_attn(kfT, ksT, vfS, vsS, qfT, qsT, gfT, negc, cT3, mix_g, oT)
        if "D" in phases:
            self.phase_outproj(oT, w_out, hown, y1)
        if "E" in phases:
            self.phase_xa_moe(y1, mem_in, ln_mix_g, ln_mix_b, mem_ln_g, mem_ln_b, xa_wq, xa_wkv, xa_wo, ln_xa_g, ln_xa_b,
                              router_w, router_b, w_up, b_up, w_down, b_down, ln_moe_g, ln_moe_b, h2s, out_t, out_regs,
                              featvec)
        allregs = list(out_regs)
        for tn in (kfT, ksT, vfS, vsS, qfT, qsT, gfT, hown, oT, y1, h2s, self.h2chk, self.h2fin):
            allregs += tn.regs
        P.wait_all("sp", allregs)
        P.emit()
        return nc

    def ln_rows(self, es_, name):
        d = dict(
            st=self.sb(es_, name + "_st", [128, 8, 6], F32),
            mv=self.sb(es_, name + "_mv", [128, 2], F32),
            sd=self.sb(es_, name + "_sd", [128, 1], F32),
            rs=self.sb(es_, name + "_rs", [128, 1], F32),
            nm=self.sb(es_, name + "_nm", [128, 1], F32),
        )
        return d

    def ln_tile(self, xt, tmp, eps=LN_EPS, gbc=None, bbc=None):
        P = self.P
        st, mv, sd, rs, nm = tmp["st"], tmp["mv"], tmp["sd"], tmp["rs"], tmp["nm"]

        def f_stats(e):
            for c in range(8):
                i = e.bn_stats(out=st.t[:, c, :], in_=xt.t[:, c * 512:(c + 1) * 512])
            return i
        P.op("dve", f_stats, reads=[xt.R], writes=[st.R])
        P.op("dve", lambda e: e.bn_aggr(out=mv.t[:], in_=st.t[:].rearrange("p a b -> p (a b)")), reads=[st.R], writes=[mv.R])
        eps_ap = self.eps_tile(eps)
        P.op("act", lambda e: e.activation(out=sd.t[:], in_=mv.t[:, 1:2], func=AF.Sqrt, bias=eps_ap, scale=1.0),
             reads=[mv.R, self._eps["eps%g" % eps].R], writes=[sd.R])
        P.op("dve", lambda e: e.reciprocal(out=rs.t[:], in_=sd.t[:]), reads=[sd.R], writes=[rs.R])
        P.op("dve", lambda e: e.tensor_scalar(out=nm.t[:], in0=mv.t[:, 0:1], scalar1=rs.t[:, 0:1], scalar2=-1.0,
                                              op0=ALU.mult, op1=ALU.mult), reads=[mv.R, rs.R], writes=[nm.R])
        P.op("act", lambda e: e.activation(out=xt.t[:], in_=xt.t[:], func=AF.Identity, bias=nm.t[:, 0:1], scale=rs.t[:, 0:1]),
             reads=[xt.R, nm.R, rs.R], writes=[xt.R])
        if gbc is not None:
            P.op("dve", lambda e: e.tensor_tensor(out=xt.t[:], in0=xt.t[:], in1=gbc.t[:], op=ALU.mult), reads=[xt.R, gbc.R], writes=[xt.R])
            P.op("dve", lambda e: e.tensor_tensor(out=xt.t[:], in0=xt.t[:], in1=bbc.t[:], op=ALU.add), reads=[xt.R, bbc.R], writes=[xt.R])

    def eps_tile(self, eps):
        return self._eps["eps%g" % eps].t[:, 0:1]

    def transpose_tile(self, xt, dstT, dst_reg, t0, banks, gT=None, bT=None, dt_out=BF16, evac=("dve", "act")):
        P = self.P
        ident_f = self.consts["ident_f"]
        for g4 in range(8):
            bank = self.next_bank(banks)

            def f_tr(e, g4=g4, bank=bank):
                for i in range(4):
                    kc = g4 * 4 + i
                    ins = e.transpose(out=bank.t[:, i * 128:(i + 1) * 128], in_=xt.t[:, kc * 128:(kc + 1) * 128], identity=ident_f.t[:])
                return ins
            P.op("pe", f_tr, reads=[xt.R, ident_f.R], writes=[bank.R])
            eng = evac[g4 % len(evac)]
            if gT is None:
                src = bank.t[:].rearrange("p (a b) -> p a b", a=4)
                dst = dstT.t[:, g4 * 4:(g4 + 1) * 4, t0:t0 + 128]
                if eng == "act":
                    P.op("act", lambda e, s=src, d=dst: e.activation(out=d, in_=s, func=AF.Copy), reads=[bank.R], writes=[dst_reg])
                else:
                    P.op("dve", lambda e, s=src, d=dst: e.tensor_copy(out=d, in_=s), reads=[bank.R], writes=[dst_reg])
            else:
                def f_ev(e, g4=g4, bank=bank, eng=eng):
                    for i in range(4):
                        kc = g4 * 4 + i
                        d = dstT.t[:, kc, t0:t0 + 128]
                        s = bank.t[:, i * 128:(i + 1) * 128]
                        if eng == "act":
                            ins = e.activation(out=d, in_=s, func=AF.Identity, bias=bT.t[:, kc:kc + 1], scale=gT.t[:, kc:kc + 1])
                        else:
                            ins = e.tensor_scalar(out=d, in0=s, scalar1=gT.t[:, kc:kc + 1], scalar2=bT.t[:, kc:kc + 1],
                                                  op0=ALU.mult, op1=ALU.add)
                    return ins
                P.op(eng, f_ev, reads=[bank.R, gT.R, bT.R], writes=[dst_reg])

    def load_w(self, wb, src_view, col0, ncols, kc0=0, kc1=KC):
        mid = (kc0 + kc1) // 2
        for a, b in ((kc0, mid), (mid, kc1)):
            if b > a:
                self.load("pool", wb.t[:, a:b, 0:ncols], src_view[:, a:b, col0:col0 + ncols], [], [wb.R], max_dma_last_dim=4096)

    def gemm_fm(self, wbufs, src_view, col0, nheads, xT, xregs, ntok, banks, epilogue, nk=KC, pre=None):
        P = self.P
        wb = wbufs[self._wrr % len(wbufs)]
        self._wrr += 1
        self.load_w(wb, src_view, col0, nheads * 128, 0, nk)
        for hh in range(nheads):
            for (c0, c1) in _ceil_chunks(0, ntok, 512):
                bank = self.next_bank(banks)

                def f_mm(e, hh=hh, c0=c0, c1=c1, bank=bank, wb=wb):
                    for kc in range(nk):
                        ins = e.matmul(bank.t[:, 0:c1 - c0], lhsT=wb.t[:, kc, hh * 128:(hh + 1) * 128], rhs=xT.t[:, kc, c0:c1],
                                       start=(kc == 0), stop=(kc == nk - 1))
                    return ins
                P.op("pe", f_mm, reads=[wb.R] + [xregs[i] for i in range(c0 // 128, (c1 + 127) // 128)], writes=[bank.R])
                epilogue(hh, c0, c1, bank)

    def gemm_tm(self, wbufs, src_view, col0, ncols, xT, xregs, ntiles, banks, epilogue, nk=KC):
        P = self.P
        wb = wbufs[self._wrr % len(wbufs)]
        self._wrr += 1
        self.load_w(wb, src_view, col0, ncols, 0, nk)
        for tt in range(ntiles):
            bank = self.next_bank(banks)

            def f_mm(e, tt=tt, bank=bank, wb=wb):
                for kc in range(nk):
                    ins = e.matmul(bank.t[:, 0:ncols], lhsT=xT.t[:, kc, tt * 128:(tt + 1) * 128], rhs=wb.t[:, kc, 0:ncols],
                                   start=(kc == 0), stop=(kc == nk - 1))
                return ins
            P.op("pe", f_mm, reads=[wb.R, xregs[tt]], writes=[bank.R])
            epilogue(tt, bank)

    def phase_inproj(self, x_seq, x_own, ln_g, ln_b, w_in_v, b_f, fq_g, fk_g, kfT, ksT, vfS, vsS, qfT, qsT, gfT, hown,
                     negc, lfseq, cT3, featvec):
        self._wrr = 0
        for side in ("seq", "own"):
            self.inproj_side(side, x_seq, x_own, ln_g, ln_b, w_in_v, b_f, fq_g, fk_g, kfT, ksT, vfS, vsS, qfT, qsT, gfT, hown,
                             negc, lfseq, cT3, featvec)

    def inproj_side(self, side, x_seq, x_own, ln_g, ln_b, w_in_v, b_f, fq_g, fk_g, kfT, ksT, vfS, vsS, qfT, qsT, gfT, hown,
                    negc, lfseq, cT3, featvec):
        nc, P = self.nc, self.P
        C = self.consts
        psum = self.psum
        if True:
            ntiles = NB if side == "seq" else NQ
            ntok = ntiles * 128
            xsrc = x_seq if side == "seq" else x_own
            with self.scope() as es1:
                hT = self.sb(es1, "hT_" + side, [128, KC, ntok], BF16, ntiles)
                with self.scope() as es2:
                    xts = [self.sb(es2, "xt%d" % i, [128, D], F32) for i in range(2)]
                    tmp = self.ln_rows(es2, "lnA")
                    if side == "seq":
                        gT = featvec(es2, "gT_in", ln_g)
                        bT = featvec(es2, "bT_in", ln_b)
                        gbc = bbc = None
                    else:
                        gT = bT = None
                        gbc = self.sb(es2, "gbc", [128, D], F32)
                        bbc = self.sb(es2, "bbc", [128, D], F32)
                        self.load("sp", gbc.t[:], ln_g.ap().partition_broadcast(128), [], [gbc.R])
                        self.load("sp", bbc.t[:], ln_b.ap().partition_broadcast(128), [], [bbc.R])
                    for tt in range(ntiles):
                        xt = xts[tt % 2]
                        self.load("sp", xt.t[:], xsrc.ap()[tt * 128:(tt + 1) * 128, :], [], [xt.R])
                        self.ln_tile(xt, tmp, LN_EPS, gbc, bbc)
                        if side == "own":
                            self.load("sp", hown.t.ap()[tt * 128:(tt + 1) * 128, :], xt.t[:], [xt.R], [hown.regs[tt]])
                        self.transpose_tile(xt, hT, hT.regs[tt], tt * 128, psum[0:4], gT, bT)
                with self.scope() as es2:
                    wbufs = [self.sb(es2, "wb%d" % i, [128, KC, 256], BF16) for i in range(2)]
                    stage = [self.sb(es2, "stg%d" % i, [128, ntok], BF16) for i in range(2)]
                    stagef = [self.sb(es2, "stgf%d" % i, [128, T], F32) for i in range(2)] if side == "own" else None
                    vstage = [self.sb(es2, "vstg%d" % i, [128, NB, 256], BF16) for i in range(2)] if side == "seq" else None
                    sqb = [self.sb(es2, "sqb%d" % i, [128, 512], BF16) for i in range(2)]
                    lnv = [self.sb(es2, "lnv%d" % i, [128, 512], F32) for i in range(2)]
                    gk = featvec(es2, "gk", fk_g, 1)
                    gq = featvec(es2, "gq", fq_g, 1)
                    P.op("dve", lambda e: e.tensor_scalar(out=gq.t[:], in0=gq.t[:], scalar1=SCALE, scalar2=None, op0=ALU.mult),
                         reads=[gq.R], writes=[gq.R])
                    bfb = self.sb(es2, "bfb", [128, NFH], F32)
                    self.load("sp", bfb.t[:], b_f.ap().partition_broadcast(128), [], [bfb.R])
                    mm_banks = psum[0:4]
                    aux_banks = psum[4:6]
                    self._rr = 0
                    eps_rms = self.eps_tile(RMS_EPS)
                    one_ap = self.eps_tile(1.0)

                    def rms_epi(dst_scr, head0, gvec):
                        def epi(hh, c0, c1, bank):
                            i = self._rr % 2
                            self._rr += 1
                            w = c1 - c0
                            st = stage[(head0 + hh) % 2]
                            P.op("act", lambda e: e.activation(out=sqb[i].t[:, 0:w], in_=bank.t[:, 0:w], func=AF.Square),
                                 reads=[bank.R], writes=[sqb[i].R])
                            b2 = self.next_bank(aux_banks)
                            P.op("pe", lambda e: e.matmul(b2.t[:, 0:w], lhsT=C["ones_b"].t[:], rhs=sqb[i].t[:, 0:w], start=True, stop=True),
                                 reads=[sqb[i].R, C["ones_b"].R], writes=[b2.R])
                            P.op("act", lambda e: e.activation(out=lnv[i].t[:, 0:w], in_=b2.t[:, 0:w], func=AF.Ln,
                                                               bias=eps_rms, scale=1.0 / HD),
                                 reads=[b2.R, self._eps["eps%g" % RMS_EPS].R], writes=[lnv[i].R])
                            P.op("act", lambda e: e.activation(out=lnv[i].t[:, 0:w], in_=lnv[i].t[:, 0:w], func=AF.Exp, scale=-0.5),
                                 reads=[lnv[i].R], writes=[lnv[i].R])
                            P.op("dve", lambda e: e.scalar_tensor_tensor(out=st.t[:, c0:c1], in0=bank.t[:, 0:w], scalar=gvec.t[:, 0:1],
                                                                         in1=lnv[i].t[:, 0:w], op0=ALU.mult, op1=ALU.mult),
                                 reads=[bank.R, lnv[i].R, gvec.R], writes=[st.R])
                            if c1 == ntok:
                                h = head0 + hh
                                self.load("sp", dst_scr.t.ap()[h], st.t[:, 0:ntok], [st.R], [dst_scr.regs[h]])
                        return epi

                    def copy_epi(dst_scr, head0, scale, eng):
                        def epi(hh, c0, c1, bank):
                            w = c1 - c0
                            st = stage[(head0 + hh) % 2]
                            if eng == "act":
                                P.op("act", lambda e: e.activation(out=st.t[:, c0:c1], in_=bank.t[:, 0:w], func=AF.Copy, scale=scale),
                                     reads=[bank.R], writes=[st.R])
                            else:
                                P.op("dve", lambda e: e.tensor_scalar(out=st.t[:, c0:c1], in0=bank.t[:, 0:w], scalar1=scale, scalar2=None,
                                                                      op0=ALU.mult), reads=[bank.R], writes=[st.R])
                            if c1 == ntok:
                                h = head0 + hh
                                self.load("sp", dst_scr.t.ap()[h], st.t[:, 0:ntok], [st.R], [dst_scr.regs[h]])
                        return epi

                    def sig_epi(dst_scr, head0):
                        def epi(hh, c0, c1, bank):
                            w = c1 - c0
                            st = stagef[(head0 + hh) % 2]
                            P.op("act", lambda e: e.activation(out=st.t[:, c0:c1], in_=bank.t[:, 0:w], func=AF.Sigmoid),
                                 reads=[bank.R], writes=[st.R])
                            if c1 == ntok:
                                h = head0 + hh
                                self.load("sp", dst_scr.t.ap()[h], st.t[:, 0:ntok], [st.R], [dst_scr.regs[h]])
                        return epi

                    def v_epi(dst_scr, chunk):
                        vs_ = vstage[chunk % 2]

                        def epi(tt, bank):
                            eng = "dve" if tt % 2 == 0 else "act"
                            if eng == "act":
                                P.op("act", lambda e: e.activation(out=vs_.t[:, tt, :], in_=bank.t[:, 0:256], func=AF.Copy),
                                     reads=[bank.R], writes=[vs_.R])
                            else:
                                P.op("dve", lambda e: e.tensor_copy(out=vs_.t[:, tt, :], in_=bank.t[:, 0:256]), reads=[bank.R], writes=[vs_.R])
                            if tt == NB - 1:
                                dv = dst_scr.t.ap().rearrange("(t p) c -> p t c", p=128)[:, :, chunk * 256:(chunk + 1) * 256]
                                self.load("sp", dv, vs_.t[:], [vs_.R], [dst_scr.regs[chunk]])
                        return epi

                    if side == "seq":
                        for pr in range(8):
                            self.gemm_fm(wbufs, w_in_v, KF + pr * 256, 2, hT, hT.regs, ntok, mm_banks, rms_epi(kfT, pr * 2, gk))
                        for pr in range(8):
                            self.gemm_fm(wbufs, w_in_v, KS + pr * 256, 2, hT, hT.regs, ntok, mm_banks,
                                         copy_epi(ksT, pr * 2, 1.0, "act" if pr % 2 else "dve"))
                        for ch in range(8):
                            self.gemm_tm(wbufs, w_in_v, VF + ch * 256, 256, hT, hT.regs, NB, mm_banks, v_epi(vfS, ch))
                        for ch in range(8):
                            self.gemm_tm(wbufs, w_in_v, VS + ch * 256, 256, hT, hT.regs, NB, mm_banks, v_epi(vsS, ch))
                    else:
                        for pr in range(8):
                            self.gemm_fm(wbufs, w_in_v, QF + pr * 256, 2, hT, hT.regs, ntok, mm_banks, rms_epi(qfT, pr * 2, gq))
                        for pr in range(8):
                            self.gemm_fm(wbufs, w_in_v, QS + pr * 256, 2, hT, hT.regs, ntok, mm_banks,
                                         copy_epi(qsT, pr * 2, SCALE, "act" if pr % 2 else "dve"))
                        for pr in range(8):
                            self.gemm_fm(wbufs, w_in_v, GF + pr * 256, 2, hT, hT.regs, ntok, mm_banks, sig_epi(gfT, pr * 2))

                    lfown = self.sb(es2, "lfown", [128, NQ, NFH], F32)
                    cown = self.sb(es2, "cown", [128, NQ, NFH], F32)
                    xf = self.sb(es2, "xf", [128, NFH], F32)
                    lf_dst = lfseq if side == "seq" else lfown

                    def f_epi(tt, bank):
                        P.op("dve", lambda e: e.tensor_tensor(out=xf.t[:], in0=bank.t[:, 0:NFH], in1=bfb.t[:], op=ALU.add),
                             reads=[bank.R, bfb.R], writes=[xf.R])
                        P.op("act", lambda e: e.activation(out=xf.t[:], in_=xf.t[:], func=AF.Exp, scale=-1.0), reads=[xf.R], writes=[xf.R])
                        P.op("act", lambda e: e.activation(out=xf.t[:], in_=xf.t[:], func=AF.Ln, bias=one_ap, scale=1.0),
                             reads=[xf.R, self._eps["eps%g" % 1.0].R], writes=[xf.R])
                        P.op("dve", lambda e: e.tensor_scalar(out=lf_dst.t[:, tt, :], in0=xf.t[:], scalar1=-1.0, scalar2=None, op0=ALU.mult),
                             reads=[xf.R], writes=[lf_dst.R])
                    self.gemm_tm(wbufs, w_in_v, FL, NFH, hT, hT.regs, ntiles, mm_banks, f_epi)
                    for tt in range(ntiles):
                        bank = self.next_bank(aux_banks)

                        def f_cs(e, tt=tt, bank=bank):
                            terms = [(C["tri_f"], lf_dst.t[:, tt, :])]
                            for t2 in range(tt):
                                terms.append((C["ones_f"], lf_dst.t[:, t2, :]))
                            if side == "own":
                                for t2 in range(8):
                                    terms.append((C["jones"], lfseq.t[:, t2, :]))
                            for n, (l, r) in enumerate(terms):
                                ins = e.matmul(bank.t[:, 0:NFH], lhsT=l.t[:], rhs=r, start=(n == 0), stop=(n == len(terms) - 1))
                            return ins
                        P.op("pe", f_cs, reads=[lf_dst.R, lfseq.R, C["tri_f"].R, C["ones_f"].R, C["jones"].R], writes=[bank.R])
                        if side == "seq":
                            P.op("dve", lambda e, tt=tt, bank=bank: e.tensor_scalar(out=negc.t[:, tt, :], in0=bank.t[:, 0:NFH], scalar1=-1.0,
                                                                                   scalar2=None, op0=ALU.mult), reads=[bank.R], writes=[negc.R])
                        else:
                            P.op("dve", lambda e, tt=tt, bank=bank: e.tensor_copy(out=cown.t[:, tt, :], in_=bank.t[:, 0:NFH]),
                                 reads=[bank.R], writes=[cown.R])
                    if side == "own":
                        cTf = self.sb(es2, "cTf", [NFH, T], F32)
                        r1 = self.sb(es2, "r1", [NFH, T], F32)
                        hi = self.sb(es2, "hi", [NFH, T], BF16)
                        P.op("dve", lambda e: e.memset(cT3.t[:], 0.0), writes=[cT3.R])
                        for tt in range(NQ):
                            bank = self.next_bank(aux_banks)
                            P.op("pe", lambda e, tt=tt, bank=bank: e.transpose(out=bank.t[0:NFH, 0:128], in_=cown.t[:, tt, :], identity=C["ident_f"].t[:]),
                                 reads=[cown.R, C["ident_f"].R], writes=[bank.R])
                            P.op("dve", lambda e, tt=tt, bank=bank: e.tensor_copy(out=cTf.t[:, tt * 128:(tt + 1) * 128], in_=bank.t[0:NFH, 0:128]),
                                 reads=[bank.R], writes=[cTf.R])
                        P.op("dve", lambda e: e.tensor_copy(out=cT3.t[0:NFH, :], in_=cTf.t[:]), reads=[cTf.R], writes=[cT3.R])
                        P.op("dve", lambda e: e.tensor_tensor(out=r1.t[:], in0=cTf.t[:], in1=cT3.t[0:NFH, :], op=ALU.subtract),
                             reads=[cTf.R, cT3.R], writes=[r1.R])
                        P.op("dve", lambda e: e.tensor_copy(out=cT3.t[32:32 + NFH, :], in_=r1.t[:]), reads=[r1.R], writes=[cT3.R])
                        P.op("dve", lambda e: e.tensor_copy(out=hi.t[:], in_=r1.t[:]), reads=[r1.R], writes=[hi.R])
                        P.op("dve", lambda e: e.tensor_tensor(out=r1.t[:], in0=r1.t[:], in1=hi.t[:], op=ALU.subtract),
                             reads=[r1.R, hi.R], writes=[r1.R])
                        P.op("dve", lambda e: e.tensor_copy(out=cT3.t[64:64 + NFH, :], in_=r1.t[:]), reads=[r1.R], writes=[cT3.R])

    @staticmethod
    def unit_type(s, kb):
        if s > kb:
            return None
        if s == kb:
            return 0
        if s == kb - 8:
            return 2
        return 1

    @staticmethod
    def att_chunks(kb):
        s0 = max(0, kb - 8)
        a0 = s0 * 128
        out = []
        if a0 < 512:
            out.append((a0, 512))
        out.append((max(a0, 512), 1024))
        return out

    def phase_attn(self, kfT, ksT, vfS, vsS, qfT, qsT, gfT, negc, cT3, mix_g, oT):
        nc, P = self.nc, self.P
        C = self.consts
        ps = self.psum
        with self.scope() as es1:
            kT = [self.sb(es1, "kT%d" % i, [128, S], BF16) for i in range(2)]
            qT = [self.sb(es1, "qT%d" % i, [128, T], BF16) for i in range(2)]
            qn = [self.sb(es1, "qn%d" % i, [128, T], BF16) for i in range(2)]
            vv = [self.sb(es1, "vv%d" % i, [128, NB, 128], BF16) for i in range(2)]
            gg = [self.sb(es1, "gg%d" % i, [128, T], F32) for i in range(2)]
            pt = [self.sb(es1, "pt%d" % i, [128, 512], BF16) for i in range(3)]
            et = [self.sb(es1, "et%d" % i, [128, 512], F32) for i in range(2)]
            spt = [self.sb(es1, "spt%d" % i, [128, 512], F32) for i in range(2)]
            Racc = self.sb(es1, "Racc", [128, T], F32, 2)
            oc = self.sb(es1, "oc", [128, T], F32, 2)
            denc = self.sb(es1, "denc", [128, T], F32, 2)
            sq = self.sb(es1, "sq", [128, T], BF16, 2)
            tot = self.sb(es1, "tot", [128, T], F32, 2)
            ostg = [self.sb(es1, "ostg%d" % i, [128, T], BF16) for i in range(2)]
            zer = self.sb(es1, "zer", [128, 512], BF16)
            mixgT = self.featvec(es1, "mixgT", mix_g, 32)
            P.op("dve", lambda e: e.memset(zer.t[:], 0.0), writes=[zer.R])
            eps_rms = self.eps_tile(RMS_EPS)
            one_ap = self.eps_tile(1.0)
            epsR = self._eps["eps%g" % RMS_EPS].R
            oneR = self._eps["eps%g" % 1.0].R
            OT = [ps[4], ps[5]]
            DEN = [ps[6], ps[7]]
            self._prr = 0

            def post(h_out, is_fox, slot):
                st = ostg[h_out % 2]
                for half in range(2):
                    c0, c1 = half * 512, (half + 1) * 512
                    R2 = [oc.regs[half]]
                    P.op("act", lambda e, c0=c0, c1=c1: e.activation(out=sq.t[:, c0:c1], in_=oc.t[:, c0:c1], func=AF.Square),
                         reads=R2, writes=[sq.regs[half]])
                    bank = ps[3]
                    P.op("pe", lambda e, c0=c0, c1=c1, bank=bank: e.matmul(bank.t[:, 0:512], lhsT=C["ones_b"].t[:], rhs=sq.t[:, c0:c1],
                                                                         start=True, stop=True),
                         reads=[sq.regs[half], C["ones_b"].R], writes=[bank.R])
                    if is_fox:
                        P.op("act", lambda e, c0=c0, c1=c1: e.activation(out=denc.t[:, c0:c1], in_=denc.t[:, c0:c1], func=AF.Square,
                                                                         scale=RMS_EPS ** 0.5),
                             reads=[denc.regs[half]], writes=[denc.regs[half]])
                        P.op("dve", lambda e, c0=c0, c1=c1, bank=bank: e.scalar_tensor_tensor(
                            out=tot.t[:, c0:c1], in0=bank.t[:, 0:512], scalar=1.0 / HD, in1=denc.t[:, c0:c1], op0=ALU.mult, op1=ALU.add),
                            reads=[bank.R, denc.regs[half]], writes=[tot.regs[half]])
                        P.op("act", lambda e, c0=c0, c1=c1: e.activation(out=tot.t[:, c0:c1], in_=tot.t[:, c0:c1], func=AF.Ln),
                             reads=[tot.regs[half]], writes=[tot.regs[half]])
                    else:
                        P.op("act", lambda e, c0=c0, c1=c1, bank=bank: e.activation(out=tot.t[:, c0:c1], in_=bank.t[:, 0:512], func=AF.Ln,
                                                                                    bias=eps_rms, scale=1.0 / HD),
                             reads=[bank.R, epsR], writes=[tot.regs[half]])
                    P.op("act", lambda e, c0=c0, c1=c1: e.activation(out=tot.t[:, c0:c1], in_=tot.t[:, c0:c1], func=AF.Exp, scale=-0.5),
                         reads=[tot.regs[half]], writes=[tot.regs[half]])
                    if is_fox:
                        P.op("dve", lambda e, c0=c0, c1=c1: e.scalar_tensor_tensor(
                            out=oc.t[:, c0:c1], in0=oc.t[:, c0:c1], scalar=mixgT.t[:, h_out:h_out + 1], in1=tot.t[:, c0:c1],
                            op0=ALU.mult, op1=ALU.mult), reads=[oc.regs[half], tot.regs[half], mixgT.R], writes=[oc.regs[half]])
                        P.op("dve", lambda e, c0=c0, c1=c1: e.tensor_tensor(out=st.t[:, c0:c1], in0=oc.t[:, c0:c1], in1=gg[slot].t[:, c0:c1],
                                                                            op=ALU.mult),
                             reads=[oc.regs[half], gg[slot].R], writes=[st.R])
                    else:
                        P.op("dve", lambda e, c0=c0, c1=c1: e.scalar_tensor_tensor(
                            out=st.t[:, c0:c1], in0=oc.t[:, c0:c1], scalar=mixgT.t[:, h_out:h_out + 1], in1=tot.t[:, c0:c1],
                            op0=ALU.mult, op1=ALU.mult), reads=[oc.regs[half], tot.regs[half], mixgT.R], writes=[st.R])
                self.load("sp", oT.t.ap()[h_out], st.t[:], [st.R], [oT.regs[h_out]])

            for hh in range(NFH + NSH):
                is_fox = hh < NFH
                h = hh if is_fox else hh - NFH
                sl = hh % 2
                k_scr, q_scr, v_scr = (kfT, qfT, vfS) if is_fox else (ksT, qsT, vsS)
                self.load("sp", kT[sl].t[:], k_scr.t.ap()[h], [k_scr.regs[h]], [kT[sl].R])
                self.load("sp", qT[sl].t[:], q_scr.t.ap()[h], [q_scr.regs[h]], [qT[sl].R])
                self.load("sp", vv[sl].t[:], v_scr.t.ap().rearrange("(b p) c -> p b c", p=128)[:, :, h * 128:(h + 1) * 128],
                          [v_scr.regs[h // 2]], [vv[sl].R])
                if is_fox:
                    self.load("sp", gg[sl].t[:], gfT.t.ap()[h], [gfT.regs[h]], [gg[sl].R])
                else:
                    P.op("pool", lambda e, sl=sl: e.tensor_scalar(out=qn[sl].t[:], in0=qT[sl].t[:], scalar1=-1.0, scalar2=None, op0=ALU.mult),
                         reads=[qT[sl].R], writes=[qn[sl].R])
                    P.op("pool", lambda e: e.memset(Racc.t[:], 0.0), writes=[Racc.regs[0], Racc.regs[1]])
                    for b in range(2):
                        P.op("pe", lambda e, b=b, sl=sl: e.matmul(OT[b].t[:, 0:512], lhsT=vv[sl].t[:, 0, :], rhs=zer.t[:, 0:512], start=True, stop=False,
                                                                  skip_group_check=True),
                             reads=[vv[sl].R, zer.R], writes=[OT[b].R])
                kbs = range(NB) if is_fox else range(NB - 1, -1, -1)
                for kb in kbs:
                    for (c0, c1) in self.att_chunks(kb):
                        w = c1 - c0
                        half = 0 if c0 < 512 else 1
                        base = half * 512
                        slots = range(c0 // 128, c1 // 128)
                        masked = [(s, self.unit_type(s, kb)) for s in slots if self.unit_type(s, kb) is not None]
                        if is_fox:
                            sb_ = ps[self._prr % 3]
                            self._prr += 1
                            pti = pt[self._prr % 3]

                            def f_s(e, kb=kb, c0=c0, c1=c1, w=w, sb_=sb_, sl=sl, h=h, masked=masked):
                                e.matmul(sb_.t[:, 0:w], lhsT=kT[sl].t[:, kb * 128:(kb + 1) * 128], rhs=qT[sl].t[:, c0:c1], start=True, stop=False,
                                         skip_group_check=True)
                                ins = e.matmul(sb_.t[:, 0:w], lhsT=C["sel96"].t[:, h * 128:(h + 1) * 128], rhs=cT3.t[:, c0:c1], start=False,
                                               stop=(len(masked) == 0), skip_group_check=True)
                                for n, (s, ty) in enumerate(masked):
                                    o = s * 128 - c0
                                    ins = e.matmul(sb_.t[:, o:o + 128], lhsT=C["ident_b"].t[:], rhs=C["mbn"].t[:, ty, :], start=False,
                                                   stop=(n == len(masked) - 1), skip_group_check=True)
                                return ins
                            P.op("pe", f_s, reads=[kT[sl].R, qT[sl].R, C["sel96"].R, cT3.R, C["ident_b"].R, C["mbn"].R], writes=[sb_.R])
                            P.op("act", lambda e, w=w, sb_=sb_, pti=pti, kb=kb, h=h: e.activation(
                                out=pti.t[:, 0:w], in_=sb_.t[:, 0:w], func=AF.Exp, bias=negc.t[:, kb, h:h + 1], scale=1.0),
                                reads=[sb_.R, negc.R], writes=[pti.R])

                            def f_pv(e, kb=kb, c0=c0, w=w, base=base, half=half, pti=pti, sl=sl):
                                e.matmul(OT[half].t[:, c0 - base:c0 - base + w], lhsT=vv[sl].t[:, kb, :], rhs=pti.t[:, 0:w], start=(kb == 0),
                                         stop=(kb == NB - 1), skip_group_check=True)
                                return e.matmul(DEN[half].t[:, c0 - base:c0 - base + w], lhsT=C["ones_b"].t[:], rhs=pti.t[:, 0:w], start=(kb == 0),
                                                stop=(kb == NB - 1), skip_group_check=True)
                            P.op("pe", f_pv, reads=[pti.R, vv[sl].R, C["ones_b"].R], writes=[OT[half].R, DEN[half].R])
                        else:
                            zb = ps[self._prr % 2]
                            tb = ps[2 + (self._prr % 2)]
                            eti = et[self._prr % 2]
                            spi = spt[self._prr % 2]
                            ati = pt[self._prr % 3]
                            self._prr += 1
                            P.op("pe", lambda e, kb=kb, c0=c0, c1=c1, w=w, zb=zb, sl=sl: e.matmul(
                                zb.t[:, 0:w], lhsT=kT[sl].t[:, kb * 128:(kb + 1) * 128], rhs=qT[sl].t[:, c0:c1], start=True, stop=True),
                                reads=[kT[sl].R, qT[sl].R], writes=[zb.R])
                            P.op("act", lambda e, w=w, zb=zb, eti=eti: e.activation(out=eti.t[:, 0:w], in_=zb.t[:, 0:w], func=AF.Exp),
                                 reads=[zb.R], writes=[eti.R])
                            P.op("act", lambda e, w=w, eti=eti, spi=spi: e.activation(out=spi.t[:, 0:w], in_=eti.t[:, 0:w], func=AF.Ln, bias=one_ap,
                                                                                      scale=1.0),
                                 reads=[eti.R, oneR], writes=[spi.R])
                            for (s, ty) in masked:
                                o = s * 128 - c0
                                P.op("dve", lambda e, o=o, ty=ty, spi=spi: e.tensor_tensor(out=spi.t[:, o:o + 128], in0=spi.t[:, o:o + 128],
                                                                                           in1=C["m01"].t[:, ty, :], op=ALU.mult),
                                     reads=[spi.R, C["m01"].R], writes=[spi.R])

                            def f_t(e, kb=kb, c0=c0, c1=c1, w=w, tb=tb, spi=spi, sl=sl, masked=masked):
                                e.matmul(tb.t[:, 0:w], lhsT=C["uincl_f"].t[:], rhs=spi.t[:, 0:w], start=True, stop=False, skip_group_check=True)
                                e.matmul(tb.t[:, 0:w], lhsT=C["ones_f"].t[:], rhs=Racc.t[:, c0:c1], start=False, stop=False, skip_group_check=True)
                                ins = e.matmul(tb.t[:, 0:w], lhsT=kT[sl].t[:, kb * 128:(kb + 1) * 128], rhs=qn[sl].t[:, c0:c1], start=False,
                                               stop=(len(masked) == 0), skip_group_check=True)
                                for n, (s, ty) in enumerate(masked):
                                    o = s * 128 - c0
                                    ins = e.matmul(tb.t[:, o:o + 128], lhsT=C["ident_b"].t[:], rhs=C["mbp"].t[:, ty, :], start=False,
                                                   stop=(n == len(masked) - 1), skip_group_check=True)
                                return ins
                            P.op("pe", f_t, reads=[spi.R, Racc.regs[half], kT[sl].R, qn[sl].R, C["uincl_f"].R, C["ones_f"].R, C["ident_b"].R,
                                                   C["mbp"].R], writes=[tb.R])
                            P.op("pool", lambda e, c0=c0, c1=c1, w=w, spi=spi: e.tensor_tensor(out=Racc.t[:, c0:c1], in0=Racc.t[:, c0:c1],
                                                                                               in1=spi.t[:, 0:w], op=ALU.add),
                                 reads=[spi.R, Racc.regs[half]], writes=[Racc.regs[half]])
                            P.op("act", lambda e, w=w, tb=tb, ati=ati: e.activation(out=ati.t[:, 0:w], in_=tb.t[:, 0:w], func=AF.Exp, scale=-1.0),
                                 reads=[tb.R], writes=[ati.R])
                            P.op("pe", lambda e, kb=kb, c0=c0, w=w, base=base, half=half, ati=ati, sl=sl: e.matmul(
                                OT[half].t[:, c0 - base:c0 - base + w], lhsT=vv[sl].t[:, kb, :], rhs=ati.t[:, 0:w], start=False, stop=(kb == 0),
                                skip_group_check=True), reads=[ati.R, vv[sl].R], writes=[OT[half].R])
                for half in range(2):
                    c0, c1 = half * 512, (half + 1) * 512
                    P.op("dve", lambda e, c0=c0, c1=c1, half=half: e.tensor_copy(out=oc.t[:, c0:c1], in_=OT[half].t[:, 0:512]),
                         reads=[OT[half].R], writes=[oc.regs[half]])
                    if is_fox:
                        P.op("act", lambda e, c0=c0, c1=c1, half=half: e.activation(out=denc.t[:, c0:c1], in_=DEN[half].t[:, 0:512], func=AF.Copy),
                             reads=[DEN[half].R], writes=[denc.regs[half]])
                post(hh, is_fox, sl)

    def phase_outproj(self, oT, w_out, hown, y1):
        nc, P = self.nc, self.P
        ps = self.psum
        w_v = w_out.ap().rearrange("(k p) c -> p k c", p=128)
        with self.scope() as es1:
            oTs = self.sb(es1, "oTs", [128, KC, T], BF16, KC)
            wb = [self.sb(es1, "wo%d" % i, [128, KC, 512], BF16) for i in range(2)]
            hp = [self.sb(es1, "hp%d" % i, [128, 512], F32) for i in range(3)]
            ys = [self.sb(es1, "ys%d" % i, [128, 512], F32) for i in range(3)]
            for k in range(KC):
                self.load("sp", oTs.t[:, k, :], oT.t.ap()[k], [oT.regs[k]], [oTs.regs[k]])
            n = 0
            for ch in range(8):
                w = wb[ch % 2]
                for a, b in ((0, 16), (16, 32)):
                    self.load("pool", w.t[:, a:b, :], w_v[:, a:b, ch * 512:(ch + 1) * 512], [], [w.R], max_dma_last_dim=4096)
                for tt in range(NQ):
                    bank = ps[n % 4]
                    hpi, ysi = hp[n % 3], ys[n % 3]
                    n += 1
                    self.load("sp", hpi.t[:], hown.t.ap()[tt * 128:(tt + 1) * 128, ch * 512:(ch + 1) * 512], [hown.regs[tt]], [hpi.R])

                    def f_mm(e, tt=tt, bank=bank, w=w):
                        for kc in range(KC):
                            ins = e.matmul(bank.t[:, 0:512], lhsT=oTs.t[:, kc, tt * 128:(tt + 1) * 128], rhs=w.t[:, kc, :],
                                           start=(kc == 0), stop=(kc == KC - 1))
                        return ins
                    P.op("pe", f_mm, reads=[w.R] + oTs.regs, writes=[bank.R])
                    P.op("dve", lambda e, bank=bank, hpi=hpi, ysi=ysi: e.scalar_tensor_tensor(
                        out=ysi.t[:], in0=hpi.t[:], scalar=ALPHA, in1=bank.t[:, 0:512], op0=ALU.mult, op1=ALU.add),
                        reads=[bank.R, hpi.R], writes=[ysi.R])
                    self.load("sp", y1.t.ap()[tt * 128:(tt + 1) * 128, ch * 512:(ch + 1) * 512], ysi.t[:], [ysi.R], [y1.regs[tt]])

    def phase_xa_moe(self, y1, mem_in, ln_mix_g, ln_mix_b, mem_ln_g, mem_ln_b, xa_wq, xa_wkv, xa_wo, ln_xa_g, ln_xa_b,
                     router_w, router_b, w_up, b_up, w_down, b_down, ln_moe_g, ln_moe_b, h2s, out_t, out_regs, featvec):
        nc, P = self.nc, self.P
        C = self.consts
        ps = self.psum
        NEX = self.debug.get("ne", NE)
        y2s = h2s
        with self.scope() as es1:
            gbc = self.sb(es1, "gbc1", [128, D], F32)
            bbc = self.sb(es1, "bbc1", [128, D], F32)
            self.load("sp", gbc.t[:], ln_mix_g.ap().partition_broadcast(128), [], [gbc.R])
            self.load("sp", bbc.t[:], ln_mix_b.ap().partition_broadcast(128), [], [bbc.R])
            wq = self.sb(es1, "wq", [128, KC, XAW], BF16)
            wo = self.sb(es1, "wo", [128, 4, D], BF16)
            kx = self.sb(es1, "kx", [128, 4, NMEM], BF16)
            vx = self.sb(es1, "vx", [128, 2, XAW], BF16)
            wq_v = xa_wq.ap().rearrange("(k p) c -> p k c", p=128)
            wkv_v = xa_wkv.ap().rearrange("(k p) c -> p k c", p=128)
            wo_v = xa_wo.ap().rearrange("(k p) c -> p k c", p=128)
            for a, b in ((0, 16), (16, 32)):
                self.load("pool", wq.t[:, a:b, :], wq_v[:, a:b, :], [], [wq.R], max_dma_last_dim=4096)
            for a, b in ((0, 2), (2, 4)):
                for c in range(4):
                    self.load("pool", wo.t[:, a:b, c * 1024:(c + 1) * 1024], wo_v[:, a:b, c * 1024:(c + 1) * 1024], [], [wo.R], max_dma_last_dim=4096)
            tmp = self.ln_rows(es1, "lnE")
            with self.scope() as es2:
                memT = self.sb(es2, "memT", [128, KC, NMEM], BF16, 2)
                gT = featvec(es2, "gT_mem", mem_ln_g)
                bT = featvec(es2, "bT_mem", mem_ln_b)
                xm = self.sb(es2, "xm", [128, D], F32)
                wkv = self.sb(es2, "wkv", [128, KC, 512], BF16)
                for tt in range(2):
                    self.load("sp", xm.t[:], mem_in.ap()[tt * 128:(tt + 1) * 128, :], [], [xm.R])
                    self.ln_tile(xm, tmp, LN_EPS)
                    self.transpose_tile(xm, memT, memT.regs[tt], tt * 128, ps[0:4], gT, bT)
                for a, b in ((0, 16), (16, 32)):
                    self.load("pool", wkv.t[:, a:b, :], wkv_v[:, a:b, 0:512], [], [wkv.R], max_dma_last_dim=4096)
                for hx in range(4):
                    bank = ps[hx % 4]

                    def f_k(e, hx=hx, bank=bank):
                        for kc in range(KC):
                            ins = e.matmul(bank.t[:, 0:NMEM], lhsT=wkv.t[:, kc, hx * 128:(hx + 1) * 128], rhs=memT.t[:, kc, :],
                                           start=(kc == 0), stop=(kc == KC - 1))
                        return ins
                    P.op("pe", f_k, reads=[wkv.R] + memT.regs, writes=[bank.R])
                    P.op("dve", lambda e, hx=hx, bank=bank: e.tensor_copy(out=kx.t[:, hx, :], in_=bank.t[:, 0:NMEM]), reads=[bank.R], writes=[kx.R])
                for a, b in ((0, 16), (16, 32)):
                    self.load("pool", wkv.t[:, a:b, :], wkv_v[:, a:b, 512:1024], [], [wkv.R], max_dma_last_dim=4096)
                for mt in range(2):
                    bank = ps[mt % 4]

                    def f_v(e, mt=mt, bank=bank):
                        for kc in range(KC):
                            ins = e.matmul(bank.t[:, 0:512], lhsT=memT.t[:, kc, mt * 128:(mt + 1) * 128], rhs=wkv.t[:, kc, :],
                                           start=(kc == 0), stop=(kc == KC - 1))
                        return ins
                    P.op("pe", f_v, reads=[wkv.R] + memT.regs, writes=[bank.R])
                    P.op("dve", lambda e, mt=mt, bank=bank: e.tensor_copy(out=vx.t[:, mt, :], in_=bank.t[:, 0:512]), reads=[bank.R], writes=[vx.R])
            with self.scope() as es2:
                xt = self.sb(es2, "xtE", [128, D], F32)
                y2t = self.sb(es2, "y2t", [128, D], F32)
                h1T = self.sb(es2, "h1T", [128, KC, 128], BF16)
                qx = self.sb(es2, "qx", [128, 4, 128], BF16)
                pT = self.sb(es2, "pT", [128, 2, 512], BF16)
                rec = self.sb(es2, "rec", [128, 512], F32)
                xo = self.sb(es2, "xo", [128, 4, 128], BF16)
                for tt in range(NQ):
                    self.load("sp", xt.t[:], y1.t.ap()[tt * 128:(tt + 1) * 128, :], [y1.regs[tt]], [xt.R])
                    self.ln_tile(xt, tmp, LN_EPS, gbc, bbc)
                    self.transpose_tile(xt, h1T, h1T.R, 0, ps[0:2])
                    qb = ps[2]

                    def f_q(e, qb=qb):
                        for hx in range(4):
                            for kc in range(KC):
                                ins = e.matmul(qb.t[:, hx * 128:(hx + 1) * 128], lhsT=wq.t[:, kc, hx * 128:(hx + 1) * 128], rhs=h1T.t[:, kc, :],
                                               start=(kc == 0), stop=(kc == KC - 1), skip_group_check=True)
                        return ins
                    P.op("pe", f_q, reads=[wq.R, h1T.R], writes=[qb.R])
                    P.op("act", lambda e, qb=qb: e.activation(out=qx.t[:].rearrange("p a b -> p (a b)"), in_=qb.t[:, 0:512], func=AF.Copy, scale=SCALE),
                         reads=[qb.R], writes=[qx.R])
                    for mt in range(2):
                        sbk = ps[3 + mt]

                        def f_s(e, mt=mt, sbk=sbk):
                            for hx in range(4):
                                ins = e.matmul(sbk.t[:, hx * 128:(hx + 1) * 128], lhsT=kx.t[:, hx, mt * 128:(mt + 1) * 128], rhs=qx.t[:, hx, :],
                                               start=True, stop=True, skip_group_check=True)
                            return ins
                        P.op("pe", f_s, reads=[kx.R, qx.R], writes=[sbk.R])
                        P.op("act", lambda e, mt=mt, sbk=sbk: e.activation(out=pT.t[:, mt, :], in_=sbk.t[:, 0:512], func=AF.Exp), reads=[sbk.R], writes=[pT.R])
                    ob, db = ps[5], ps[6]

                    def f_o(e, ob=ob, db=db):
                        for hx in range(4):
                            for mt in range(2):
                                e.matmul(ob.t[:, hx * 128:(hx + 1) * 128], lhsT=vx.t[:, mt, hx * 128:(hx + 1) * 128], rhs=pT.t[:, mt, hx * 128:(hx + 1) * 128],
                                         start=(mt == 0), stop=(mt == 1), skip_group_check=True)
                        for mt in range(2):
                            ins = e.matmul(db.t[:, 0:512], lhsT=C["ones_b"].t[:], rhs=pT.t[:, mt, :], start=(mt == 0), stop=(mt == 1))
                        return ins
                    P.op("pe", f_o, reads=[vx.R, pT.R, C["ones_b"].R], writes=[ob.R, db.R])
                    P.op("dve", lambda e, db=db: e.reciprocal(out=rec.t[:], in_=db.t[:, 0:512]), reads=[db.R], writes=[rec.R])
                    P.op("dve", lambda e, ob=ob: e.tensor_tensor(out=xo.t[:].rearrange("p a b -> p (a b)"), in0=ob.t[:, 0:512], in1=rec.t[:], op=ALU.mult),
                         reads=[ob.R, rec.R], writes=[xo.R])
                    for ch in range(8):
                        bank = ps[ch % 2] if ch % 2 == 0 else ps[7]

                        def f_w(e, ch=ch, bank=bank):
                            for hx in range(4):
                                ins = e.matmul(bank.t[:, 0:512], lhsT=xo.t[:, hx, :], rhs=wo.t[:, hx, ch * 512:(ch + 1) * 512], start=(hx == 0), stop=(hx == 3))
                            return ins
                        P.op("pe", f_w, reads=[xo.R, wo.R], writes=[bank.R])
                        P.op("dve", lambda e, ch=ch, bank=bank: e.scalar_tensor_tensor(
                            out=y2t.t[:, ch * 512:(ch + 1) * 512], in0=xt.t[:, ch * 512:(ch + 1) * 512], scalar=ALPHA, in1=bank.t[:, 0:512],
                            op0=ALU.mult, op1=ALU.add), reads=[bank.R, xt.R], writes=[y2t.R])
                    self.load("sp", y2s.t.ap()[tt * 128:(tt + 1) * 128, :], y2t.t[:], [y2t.R], [y2s.regs[tt]])
        if self.debug.get("stop") == "E1":
            return
        for half in range(2):
            self.moe_half(half, NEX, y2s, ln_xa_g, ln_xa_b, router_w, router_b, w_up, b_up, w_down, b_down, ln_moe_g, ln_moe_b, out_t, out_regs)

    def moe_half(self, half, NEX, y2s, ln_xa_g, ln_xa_b, router_w, router_b, w_up, b_up, w_down, b_down, ln_moe_g, ln_moe_b, out_t, out_regs):
        nc, P = self.nc, self.P
        C = self.consts
        ps = self.psum
        NT = 4
        NTOK = NT * 128
        with self.scope() as es1:
            acc = self.sb(es1, "acc", [128, NT, D], F32, NT)
            h2T = self.sb(es1, "h2T", [128, KC, NTOK], BF16, NT)
            gate = self.sb(es1, "gate", [128, NT, NE], F32, NT)
            bupT = self.sb(es1, "bupT", [128, 16, NE], F32)
            with self.scope() as es2:
                gbc = self.sb(es2, "gbc2", [128, D], F32)
                bbc = self.sb(es2, "bbc2", [128, D], F32)
                self.load("sp", gbc.t[:], ln_xa_g.ap().partition_broadcast(128), [], [gbc.R])
                self.load("sp", bbc.t[:], ln_xa_b.ap().partition_broadcast(128), [], [bbc.R])
                tmp = self.ln_rows(es2, "lnF")
                xt = self.sb(es2, "xtF", [128, D], F32)
                h2Tf = self.sb(es2, "h2Tf", [128, KC, 128], F32)
                rw = self.sb(es2, "rw", [128, KC, NE], F32)
                rbb = self.sb(es2, "rbb", [128, NE], F32)
                lg = self.sb(es2, "lg", [128, NE], F32)
                top8 = self.sb(es2, "top8", [128, 8], F32)
                nmx = self.sb(es2, "nmx", [128, 1], F32)
                msk = self.sb(es2, "msk", [128, NE], F32)
                ex = self.sb(es2, "ex", [128, NE], F32)
                ssum = self.sb(es2, "ssum", [128, 1], F32)
                gTs = self.sb(es2, "gTs", [NE, NTOK], F32)
                bdn = self.sb(es2, "bdn", [NE, D], F32)
                bup = self.sb(es2, "bup", [NE, 2 * FE], F32)
                with nc.allow_non_contiguous_dma(reason="small router weight load"):
                    self.load("sp", rw.t[:], router_w.ap().rearrange("(k p) c -> p k c", p=128), [], [rw.R], allow_slow_non_contiguous=True)
                self.load("sp", rbb.t[:], router_b.ap().partition_broadcast(128), [], [rbb.R])
                self.load("sp", bdn.t[:], b_down.ap(), [], [bdn.R])
                self.load("sp", bup.t[:], b_up.ap(), [], [bup.R])
                for c in range(16):
                    bank = ps[c % 2]
                    P.op("pe", lambda e, c=c, bank=bank: e.transpose(out=bank.t[:, 0:NE], in_=bup.t[:, c * 128:(c + 1) * 128], identity=C["ident_f"].t[0:NE, 0:NE]),
                         reads=[bup.R, C["ident_f"].R], writes=[bank.R])
                    P.op("dve", lambda e, c=c, bank=bank: e.tensor_copy(out=bupT.t[:, c, :], in_=bank.t[:, 0:NE]), reads=[bank.R], writes=[bupT.R])
                if self.debug.get("stop") == "E2a":
                    return
                for tl in range(NT):
                    tt = half * NT + tl
                    self.load("sp", xt.t[:], y2s.t.ap()[tt * 128:(tt + 1) * 128, :], [y2s.regs[tt]], [xt.R])
                    self.load("sp", self.h2chk.t.ap()[tt * 128:(tt + 1) * 128, :], xt.t[:], [xt.R], [self.h2chk.regs[tt]])
                    self.ln_tile(xt, tmp, LN_EPS, gbc, bbc)
                    self.load("sp", self.h2fin.t.ap()[tt * 128:(tt + 1) * 128, :], xt.t[:], [xt.R], [self.h2fin.regs[tt]])
                    for g4 in range(8):
                        bank = ps[g4 % 4]

                        def f_tr(e, g4=g4, bank=bank):
                            for i in range(4):
                                kc = g4 * 4 + i
                                ins = e.transpose(out=bank.t[:, i * 128:(i + 1) * 128], in_=xt.t[:, kc * 128:(kc + 1) * 128], identity=C["ident_f"].t[:])
                            return ins
                        P.op("pe", f_tr, reads=[xt.R, C["ident_f"].R], writes=[bank.R])
                        src = bank.t[:].rearrange("p (a b) -> p a b", a=4)
                        P.op("act", lambda e, g4=g4, src=src: e.activation(out=h2Tf.t[:, g4 * 4:(g4 + 1) * 4, :], in_=src, func=AF.Copy),
                             reads=[bank.R], writes=[h2Tf.R])
                        P.op("dve", lambda e, g4=g4, tl=tl: e.tensor_copy(out=h2T.t[:, g4 * 4:(g4 + 1) * 4, tl * 128:(tl + 1) * 128],
                                                                         in_=h2Tf.t[:, g4 * 4:(g4 + 1) * 4, :]),
                             reads=[h2Tf.R], writes=[h2T.regs[tl]])
                    P.op("act", lambda e, tl=tl: e.activation(out=acc.t[:, tl, :], in_=xt.t[:], func=AF.Copy, scale=ALPHA), reads=[xt.R], writes=[acc.regs[tl]])
                    lb = ps[4]

                    def f_r(e, lb=lb):
                        for kc in range(KC):
                            ins = e.matmul(lb.t[:, 0:NE], lhsT=h2Tf.t[:, kc, :], rhs=rw.t[:, kc, :], start=(kc == 0), stop=(kc == KC - 1))
                        return ins
                    P.op("pe", f_r, reads=[h2Tf.R, rw.R], writes=[lb.R])
                    P.op("dve", lambda e, lb=lb: e.tensor_tensor(out=lg.t[:], in0=lb.t[:, 0:NE], in1=rbb.t[:], op=ALU.add), reads=[lb.R, rbb.R], writes=[lg.R])
                    if self.debug.get("stop") == "E2c":
                        return
                    if NEX < NE:
                        P.op("dve", lambda e: e.memset(lg.t[:, NEX:NE], -1e30), reads=[lg.R], writes=[lg.R])
                    P.op("dve", lambda e: e.max(out=top8.t[:], in_=lg.t[:]), reads=[lg.R], writes=[top8.R])
                    P.op("dve", lambda e: e.tensor_scalar(out=nmx.t[:], in0=top8.t[:, 0:1], scalar1=-1.0, scalar2=None, op0=ALU.mult), reads=[top8.R], writes=[nmx.R])
                    P.op("dve", lambda e: e.tensor_scalar(out=msk.t[:], in0=lg.t[:], scalar1=top8.t[:, 3:4], scalar2=None, op0=ALU.is_ge),
                         reads=[lg.R, top8.R], writes=[msk.R])
                    P.op("act", lambda e: e.activation(out=ex.t[:], in_=lg.t[:], func=AF.Exp, bias=nmx.t[:, 0:1], scale=1.0), reads=[lg.R, nmx.R], writes=[ex.R])
                    P.op("dve", lambda e: e.tensor_tensor(out=ex.t[:], in0=ex.t[:], in1=msk.t[:], op=ALU.mult), reads=[ex.R, msk.R], writes=[ex.R])
                    P.op("dve", lambda e: e.tensor_reduce(out=ssum.t[:], in_=ex.t[:], axis=AX.X, op=ALU.add), reads=[ex.R], writes=[ssum.R])
                    P.op("dve", lambda e: e.reciprocal(out=ssum.t[:], in_=ssum.t[:]), reads=[ssum.R], writes=[ssum.R])
                    P.op("dve", lambda e, tl=tl: e.tensor_scalar(out=gate.t[:, tl, :], in0=ex.t[:], scalar1=ssum.t[:, 0:1], scalar2=None, op0=ALU.mult),
                         reads=[ex.R, ssum.R], writes=[gate.regs[tl]])
                    if self.debug.get("stop") == "E2b":
                        return
                    gb = ps[5]
                    P.op("pe", lambda e, tl=tl, gb=gb: e.transpose(out=gb.t[0:NE, 0:128], in_=gate.t[:, tl, :], identity=C["ident_f"].t[:]),
                         reads=[gate.regs[tl], C["ident_f"].R], writes=[gb.R])
                    P.op("dve", lambda e, tl=tl, gb=gb: e.tensor_copy(out=gTs.t[:, tl * 128:(tl + 1) * 128], in_=gb.t[0:NE, 0:128]), reads=[gb.R], writes=[gTs.R])
                    for ch in range(8):
                        bank = ps[6 + ch % 2]
                        P.op("pe", lambda e, tl=tl, ch=ch, bank=bank: e.matmul(bank.t[:, 0:512], lhsT=gTs.t[:, tl * 128:(tl + 1) * 128],
                                                                               rhs=bdn.t[:, ch * 512:(ch + 1) * 512], start=True, stop=True),
                             reads=[gTs.R, bdn.R], writes=[bank.R])
                        P.op("dve", lambda e, tl=tl, ch=ch, bank=bank: e.tensor_tensor(out=acc.t[:, tl, ch * 512:(ch + 1) * 512],
                                                                                       in0=acc.t[:, tl, ch * 512:(ch + 1) * 512], in1=bank.t[:, 0:512], op=ALU.add),
                             reads=[bank.R, acc.regs[tl]], writes=[acc.regs[tl]])
            if self.debug.get("dump"):
                dg = self.dbg_t("dbg_gate", [T, NE], F32); da = self.dbg_t("dbg_acc0", [T, D], F32); db_ = self.dbg_t("dbg_bupT", [128, 16 * NE], F32)
                for tl in range(NT):
                    tt = half * NT + tl
                    self.load("sp", dg.ap()[tt * 128:(tt + 1) * 128, :], gate.t[:, tl, :], [gate.regs[tl]], [Region()])
                    self.load("sp", da.ap()[tt * 128:(tt + 1) * 128, :], acc.t[:, tl, :], [acc.regs[tl]], [Region()])
                self.load("sp", db_.ap(), bupT.t[:].rearrange("p a b -> p (a b)"), [bupT.R], [Region()])
                P.fence()
            if self.debug.get("stop") == "E2":
                return
            with self.scope() as es2:
                wu = [self.sb(es2, "wu%d" % i, [128, KC, 256], BF16) for i in range(2)]
                wd = [self.sb(es2, "wd%d" % i, [128, 8, 512], BF16) for i in range(2)]
                tg = self.sb(es2, "tg", [128, 8, NTOK], F32, 8)
                aT = [self.sb(es2, "aT%d" % i, [128, 8, NTOK], BF16, 8) for i in range(2)]
                g1 = [self.sb(es2, "g1_%d" % i, [128, NTOK], F32) for i in range(2)]
                s1 = [self.sb(es2, "s1_%d" % i, [128, NTOK], F32) for i in range(2)]
                u1 = [self.sb(es2, "u1_%d" % i, [128, NTOK], F32) for i in range(2)]
                nu = nd = nb = 0
                for ex_ in range(NEX):
                    wup_v = w_up.ap()[ex_].rearrange("(k p) c -> p k c", p=128)
                    wdn_v = w_down.ap()[ex_].rearrange("(k p) c -> p k c", p=128)
                    aTe = aT[ex_ % 2]
                    for cp in range(8):
                        w = wu[nu % 2]
                        nu += 1
                        for a, b in ((0, 16), (16, 32)):
                            self.load("pool", w.t[:, a:b, :], wup_v[:, a:b, cp * 256:(cp + 1) * 256], [], [w.R], max_dma_last_dim=4096)
                        for sub in range(2):
                            c = cp * 2 + sub
                            bank = ps[nb % 4]
                            nb += 1

                            def f_up(e, sub=sub, bank=bank, w=w):
                                for kc in range(KC):
                                    ins = e.matmul(bank.t[:, 0:NTOK], lhsT=w.t[:, kc, sub * 128:(sub + 1) * 128], rhs=h2T.t[:, kc, :],
                                                   start=(kc == 0), stop=(kc == KC - 1))
                                return ins
                            P.op("pe", f_up, reads=[w.R] + h2T.regs, writes=[bank.R])
                            i2 = c % 2
                            bcol = bupT.t[:, c, ex_:ex_ + 1]
                            if c < 8:
                                P.op("dve", lambda e, bank=bank, i2=i2, bcol=bcol: e.tensor_scalar(out=g1[i2].t[:], in0=bank.t[:, 0:NTOK], scalar1=bcol,
                                                                                                    scalar2=7.0, op0=ALU.add, op1=ALU.min),
                                     reads=[bank.R, bupT.R], writes=[g1[i2].R])
                                P.op("act", lambda e, i2=i2: e.activation(out=s1[i2].t[:], in_=g1[i2].t[:], func=AF.Sigmoid, scale=1.702),
                                     reads=[g1[i2].R], writes=[s1[i2].R])
                                P.op("dve", lambda e, i2=i2, c=c: e.tensor_tensor(out=tg.t[:, c, :], in0=g1[i2].t[:], in1=s1[i2].t[:], op=ALU.mult),
                                     reads=[g1[i2].R, s1[i2].R], writes=[tg.regs[c]])
                            else:
                                f = c - 8
                                P.op("dve", lambda e, bank=bank, i2=i2, bcol=bcol: e.tensor_scalar(out=u1[i2].t[:], in0=bank.t[:, 0:NTOK], scalar1=bcol,
                                                                                                    scalar2=7.0, op0=ALU.add, op1=ALU.min),
                                     reads=[bank.R, bupT.R], writes=[u1[i2].R])
                                P.op("dve", lambda e, i2=i2: e.tensor_scalar(out=u1[i2].t[:], in0=u1[i2].t[:], scalar1=-7.0, scalar2=1.0, op0=ALU.max, op1=ALU.add),
                                     reads=[u1[i2].R], writes=[u1[i2].R])
                                P.op("dve", lambda e, i2=i2, f=f, aTe=aTe: e.tensor_tensor(out=aTe.t[:, f, :], in0=u1[i2].t[:], in1=tg.t[:, f, :], op=ALU.mult),
                                     reads=[u1[i2].R, tg.regs[f]], writes=[aTe.regs[f]])
                    if self.debug.get("dump") and ex_ == 0 and half == 0:
                        dT = self.dbg_t("dbg_aT", [128, 8 * NTOK], BF16)
                        self.load("sp", dT.ap(), aTe.t[:].rearrange("p a b -> p (a b)"), list(aTe.regs), [Region()])
                    for ch in range(8):
                        w2 = wd[nd % 2]
                        nd += 1
                        self.load("pool", w2.t[:], wdn_v[:, :, ch * 512:(ch + 1) * 512], [], [w2.R], max_dma_last_dim=4096)
                        for tl in range(NT):
                            bank = ps[4 + nb % 4]
                            nb += 1

                            def f_dn(e, tl=tl, bank=bank, w2=w2, aTe=aTe):
                                for fc in range(8):
                                    ins = e.matmul(bank.t[:, 0:512], lhsT=aTe.t[:, fc, tl * 128:(tl + 1) * 128], rhs=w2.t[:, fc, :], start=(fc == 0), stop=(fc == 7))
                                return ins
                            P.op("pe", f_dn, reads=[w2.R] + aTe.regs, writes=[bank.R])
                            P.op("dve", lambda e, tl=tl, ch=ch, bank=bank, ex_=ex_: e.scalar_tensor_tensor(
                                out=acc.t[:, tl, ch * 512:(ch + 1) * 512], in0=bank.t[:, 0:512], scalar=gate.t[:, tl, ex_:ex_ + 1],
                                in1=acc.t[:, tl, ch * 512:(ch + 1) * 512], op0=ALU.mult, op1=ALU.add),
                                reads=[bank.R, gate.regs[tl], acc.regs[tl]], writes=[acc.regs[tl]])
            if self.debug.get("dump"):
                da1 = self.dbg_t("dbg_acc1", [T, D], F32)
                for tl in range(NT):
                    tt = half * NT + tl
                    self.load("sp", da1.ap()[tt * 128:(tt + 1) * 128, :], acc.t[:, tl, :], [acc.regs[tl]], [Region()])
                P.fence()
            with self.scope() as es2:
                gbc = self.sb(es2, "gbc3", [128, D], F32)
                bbc = self.sb(es2, "bbc3", [128, D], F32)
                self.load("sp", gbc.t[:], ln_moe_g.ap().partition_broadcast(128), [], [gbc.R])
                self.load("sp", bbc.t[:], ln_moe_b.ap().partition_broadcast(128), [], [bbc.R])
                tmp = self.ln_rows(es2, "lnO")
                xo_ = [self.sb(es2, "xout%d" % i, [128, D], F32) for i in range(2)]
                for tl in range(NT):
                    tt = half * NT + tl
                    xfin = xo_[tl % 2]
                    P.op("act", lambda e, tl=tl, xfin=xfin: e.activation(out=xfin.t[:], in_=acc.t[:, tl, :], func=AF.Copy), reads=[acc.regs[tl]], writes=[xfin.R])
                    self.ln_tile(xfin, tmp, LN_EPS, gbc, bbc)
                    self.load("sp", out_t.ap()[tt * 128:(tt + 1) * 128, :], xfin.t[:], [xfin.R], [out_regs[tt]])


def _consts(j):
    bf = ml_dtypes.bfloat16
    idx = np.arange(128)
    c = {}
    c["c_ident_f"] = np.eye(128, dtype=np.float32)
    c["c_ident_b"] = np.eye(128, dtype=np.float32).astype(bf)
    c["c_ones_b"] = np.ones((128, 128), np.float32).astype(bf)
    c["c_ones_f"] = np.ones((128, 128), np.float32)
    c["c_tri_f"] = (idx[:, None] <= idx[None, :]).astype(np.float32)
    c["c_uincl_f"] = (idx[:, None] >= idx[None, :]).astype(np.float32)
    sel = np.zeros((96, NFH * 128), np.float32)
    for h in range(NFH):
        for r in (h, 32 + h, 64 + h):
            sel[r, h * 128:(h + 1) * 128] = 1.0
    c["c_sel96"] = sel.astype(bf)
    c["c_jones"] = np.full((128, 128), float(j), np.float32)
    vis_incl = (idx[:, None] <= idx[None, :]).astype(np.float32)
    vis_strict = (idx[:, None] < idx[None, :]).astype(np.float32)
    allv = np.ones((128, 128), np.float32)
    nonev = np.zeros((128, 128), np.float32)
    fox = [vis_incl, nonev, nonev] if j == 0 else [allv, allv, vis_incl]
    sb = [vis_strict, nonev, nonev] if j == 0 else [allv, allv, vis_strict]
    c["c_mbn"] = np.stack([(1.0 - m) * NEG for m in fox], axis=1).astype(bf)
    c["c_mbp"] = np.stack([(1.0 - m) * (-NEG) for m in sb], axis=1).astype(bf)
    c["c_m01"] = np.stack(sb, axis=1).astype(np.float32)
    return c


def make_in_maps(inputs, names=None, n_cores=N_CORES):
    f = lambda a: np.ascontiguousarray(np.asarray(a, dtype=np.float32))
    shared = {
        "ln_in_g": f(inputs["ln_in_g"]), "ln_in_b": f(inputs["ln_in_b"]),
        "w_in": f(inputs["w_in"][0]), "b_f": f(inputs["b_f"][0]),
        "fox_q_norm_g": f(inputs["fox_q_norm_g"][0]), "fox_k_norm_g": f(inputs["fox_k_norm_g"][0]),
        "mix_norm_g": f(inputs["mix_norm_g"][0]), "w_out": f(inputs["w_out"][0]),
        "ln_mix_g": f(inputs["ln_mix_g"][0]), "ln_mix_b": f(inputs["ln_mix_b"][0]),
        "mem_ln_g": f(inputs["mem_ln_g"][0]), "mem_ln_b": f(inputs["mem_ln_b"][0]),
        "xa_wq": f(inputs["xa_wq"][0]), "xa_wkv": f(inputs["xa_wkv"][0]), "xa_wo": f(inputs["xa_wo"][0]),
        "ln_xa_g": f(inputs["ln_xa_g"][0]), "ln_xa_b": f(inputs["ln_xa_b"][0]),
        "router_w": f(inputs["router_w"][0]), "router_b": f(inputs["router_b"][0]),
        "w_up": f(inputs["w_up"][0]), "b_up": f(inputs["b_up"][0]),
        "w_down": f(inputs["w_down"][0]), "b_down": f(inputs["b_down"][0]),
        "ln_moe_g": f(inputs["ln_moe_g"][0]), "ln_moe_b": f(inputs["ln_moe_b"][0]),
    }
    x = np.asarray(inputs["x"], dtype=np.float32)
    mem = np.asarray(inputs["mem"], dtype=np.float32)
    maps = []
    for c in range(n_cores):
        b, j = c // 2, c % 2
        m = dict(shared)
        m["x_seq"] = np.ascontiguousarray(x[b])
        m["x_own"] = np.ascontiguousarray(x[b, j * T:(j + 1) * T])
        m["mem"] = np.ascontiguousarray(mem[b])
        m.update(_consts(j))
        if names is not None:
            m = {k: v for k, v in m.items() if k in names}
        maps.append(m)
    return maps


_NC_CACHE = {}


def kernel(**inputs):
    if "nc" not in _NC_CACHE:
        bld = Builder()
        _NC_CACHE["nc"] = bld.build()
        _NC_CACHE["names"] = set(bld.inputs.keys())
    nc = _NC_CACHE["nc"]
    maps = make_in_maps(inputs, _NC_CACHE["names"])
    res = run_bass_kernel_spmd(nc, maps, core_ids=list(range(N_CORES)))
    out = np.empty((4, S, D), np.float32)
    for c in range(N_CORES):
        b, j = c // 2, c % 2
        out[b, j * T:(j + 1) * T] = res.results[c]["out"]
    return out
```

```python
import numpy as np
import ml_dtypes
from contextlib import ExitStack, contextmanager
import concourse.bass as bass
import concourse.mybir as mybir
from concourse.bass_utils import run_bass_kernel_spmd

F32 = mybir.dt.float32
BF16 = mybir.dt.bfloat16
I32 = mybir.dt.int32
U32 = mybir.dt.uint32
AF = mybir.ActivationFunctionType
ALU = mybir.AluOpType
AX = mybir.AxisListType

D = 4096
KC = 32
S = 2048
T = 1024
NB = 16
NQ = 8
HD = 128
NFH = 16
NSH = 16
N_IN = 14352
QF, KF, VF, GF, FL, QS, KS, VS = 0, 2048, 4096, 6144, 8192, 8208, 10256, 12304
NMEM = 256
XAW = 512
NE = 32
FE = 1024
SCALE = 128 ** -0.5
ALPHA = 2 ** 0.25
LN_EPS = 1e-5
RMS_EPS = 1e-6
NEG = -30000.0
N_CORES = 8
ENG = ("pe", "act", "dve", "pool", "sp")


class Region:
    __slots__ = ("w", "r", "excl")

    def __init__(self, excl=False):
        self.w = None
        self.r = {}
        self.excl = excl


class Prog:
    def __init__(self, nc, es, n_dma_sems=40):
        self.nc = nc
        self.q = {e: [] for e in ENG}
        self.esem = {e: es.enter_context(nc.semaphore("es_" + e)) for e in ENG}
        self.ecnt = {e: 0 for e in ENG}
        self.waited = {e: {} for e in ENG}
        self.dsems = [es.enter_context(nc.semaphore("ds%d" % i)) for i in range(n_dma_sems)]
        self.dval = [0] * n_dma_sems
        self.dnext = 0
        self.nops = 0

    def _wait(self, eng, ticket):
        sem, val = ticket
        k = id(sem)
        if self.waited[eng].get(k, 0) >= val:
            return
        self.waited[eng][k] = val
        self.q[eng].append(lambda e, s=sem, v=val: e.wait_ge(s, v))

    def op(self, eng, fn, reads=(), writes=(), dma=False):
        deps = []
        for R in reads:
            if R.w is not None:
                deps.append(R.w)
            if R.excl:
                deps.extend(R.r.values())
        for R in writes:
            if R.w is not None:
                deps.append(R.w)
            deps.extend(R.r.values())
        pes = self.esem["pe"]
        for t in deps:
            if eng == "pe" and t[0] is pes:
                continue
            self._wait(eng, t)
        if dma:
            i = self.dnext
            self.dnext = (i + 1) % len(self.dsems)
            sem = self.dsems[i]
            if self.dval[i] > 0:
                self._wait(eng, (sem, self.dval[i]))
            self.dval[i] += 16
            ticket = (sem, self.dval[i])
            self.q[eng].append(lambda e, s=sem, f=fn: f(e).then_inc(s, 16))
        else:
            self.ecnt[eng] += 1
            sem = self.esem[eng]
            ticket = (sem, self.ecnt[eng])
            self.q[eng].append(lambda e, s=sem, f=fn: f(e).then_inc(s, 1))
        for R in reads:
            R.r[id(ticket[0])] = ticket
        for R in writes:
            R.w = ticket
            R.r = {}
        self.nops += 1
        return ticket

    def fence(self):
        tickets = [(self.esem[e], self.ecnt[e]) for e in ENG if self.ecnt[e] > 0]
        tickets += [(self.dsems[i], self.dval[i]) for i in range(len(self.dsems)) if self.dval[i] > 0]
        for e in ENG:
            for t in tickets:
                if t[0] is self.esem[e]:
                    continue
                self._wait(e, t)

    def wait_all(self, eng, regions):
        for R in regions:
            if R.w is not None:
                self._wait(eng, R.w)

    def emit(self):
        nc = self.nc
        allsems = list(self.esem.values()) + list(self.dsems)
        with nc.Block() as b0:
            @b0.gpsimd
            def _(e):
                for s in allsems:
                    e.sem_clear(s)
        with nc.Block() as block:
            @block.tensor
            def _(e):
                for f in self.q["pe"]:
                    f(e)

            @block.scalar
            def _(e):
                for f in self.q["act"]:
                    f(e)

            @block.vector
            def _(e):
                for f in self.q["dve"]:
                    f(e)

            @block.gpsimd
            def _(e):
                for f in self.q["pool"]:
                    f(e)

            @block.sync
            def _(e):
                for f in self.q["sp"]:
                    f(e)
        with nc.Block() as b2:
            @b2.gpsimd
            def _(e):
                for s in allsems:
                    e.sem_clear(s)


class Tn:
    def __init__(self, t, nreg=1):
        self.t = t
        self.regs = [Region() for _ in range(nreg)]

    @property
    def R(self):
        return self.regs[0]


def _ceil_chunks(c0, c1, step):
    out = []
    while c0 < c1:
        out.append((c0, min(c0 + step, c1)))
        c0 += step
    return out


class Builder:
    def __init__(self, debug=None):
        self.debug = debug or {}
        self.nc = bass.Bass("TRN2", target_bir_lowering=False)
        self.es = ExitStack()
        self.P = Prog(self.nc, self.es)
        self.inputs = {}
        self.psum_rr = 0

    def din(self, name, shape, dt=F32):
        t = self.nc.dram_tensor(name, list(shape), dt, kind="ExternalInput")
        self.inputs[name] = t
        return t

    def dscr(self, name, shape, dt, nreg=1):
        kind = "ExternalOutput" if name in self.debug.get("outs", ()) else "Internal"
        t = self.nc.dram_tensor(name, list(shape), dt, kind=kind)
        return Tn(t, nreg)

    def dbg_t(self, name, shape, dt):
        if not hasattr(self, "_dbg"):
            self._dbg = {}
        if name not in self._dbg:
            self._dbg[name] = self.nc.dram_tensor(name, list(shape), dt, kind="ExternalOutput")
        return self._dbg[name]

    def sb(self, es, name, shape, dt, nreg=1):
        self._uid = getattr(self, "_uid", 0) + 1
        t = es.enter_context(self.nc.sbuf_tensor("%s_%d" % (name, self._uid), list(shape), dt))
        return Tn(t, nreg)

    @contextmanager
    def scope(self):
        with ExitStack() as es_:
            yield es_
            self.P.fence()

    def load(self, eng, dst_ap, src_ap, reads, writes, **kw):
        return self.P.op(eng, lambda e: e.dma_start(out=dst_ap, in_=src_ap, **kw), reads=reads, writes=writes, dma=True)

    def next_bank(self, pool):
        i = self.psum_rr % len(pool)
        self.psum_rr += 1
        return pool[i]

    def build(self):
        nc, P, es = self.nc, self.P, self.es
        dbg = self.debug
        phases = dbg.get("phases", "ABCDEF")

        x_seq = self.din("x_seq", [S, D])
        x_own = self.din("x_own", [T, D])
        mem_in = self.din("mem", [NMEM, D])
        ln_in_g = self.din("ln_in_g", [D]); ln_in_b = self.din("ln_in_b", [D])
        w_in = self.din("w_in", [D, N_IN])
        b_f = self.din("b_f", [NFH])
        fq_g = self.din("fox_q_norm_g", [HD]); fk_g = self.din("fox_k_norm_g", [HD])
        mix_g = self.din("mix_norm_g", [32, HD])
        w_out = self.din("w_out", [D, D])
        ln_mix_g = self.din("ln_mix_g", [D]); ln_mix_b = self.din("ln_mix_b", [D])
        mem_ln_g = self.din("mem_ln_g", [D]); mem_ln_b = self.din("mem_ln_b", [D])
        xa_wq = self.din("xa_wq", [D, XAW]); xa_wkv = self.din("xa_wkv", [D, 2 * XAW]); xa_wo = self.din("xa_wo", [XAW, D])
        ln_xa_g = self.din("ln_xa_g", [D]); ln_xa_b = self.din("ln_xa_b", [D])
        router_w = self.din("router_w", [D, NE]); router_b = self.din("router_b", [NE])
        NEX = dbg.get("ne", NE)
        w_up = self.din("w_up", [NEX, D, 2 * FE]); b_up = self.din("b_up", [NE, 2 * FE])
        w_down = self.din("w_down", [NEX, FE, D]); b_down = self.din("b_down", [NE, D])
        ln_moe_g = self.din("ln_moe_g", [D]); ln_moe_b = self.din("ln_moe_b", [D])
        c_ident_f = self.din("c_ident_f", [128, 128])
        c_ident_b = self.din("c_ident_b", [128, 128], BF16)
        c_ones_b = self.din("c_ones_b", [128, 128], BF16)
        c_ones_f = self.din("c_ones_f", [128, 128])
        c_tri_f = self.din("c_tri_f", [128, 128])
        c_uincl_f = self.din("c_uincl_f", [128, 128])
        c_sel96 = self.din("c_sel96", [96, NFH * 128], BF16)
        c_jones = self.din("c_jones", [128, 128])
        c_mbn = self.din("c_mbn", [128, 3, 128], BF16)
        c_mbp = self.din("c_mbp", [128, 3, 128], BF16)
        c_m01 = self.din("c_m01", [128, 3, 128])
        out_t = self.nc.dram_tensor("out", [T, D], F32, kind="ExternalOutput")
        out_regs = [Region() for _ in range(NQ)]

        kfT = self.dscr("kfT", [NFH, 128, S], BF16, NFH)
        ksT = self.dscr("ksT", [NSH, 128, S], BF16, NSH)
        vfS = self.dscr("vfS", [S, NFH * HD], BF16, 8)
        vsS = self.dscr("vsS", [S, NSH * HD], BF16, 8)
        qfT = self.dscr("qfT", [NFH, 128, T], BF16, NFH)
        qsT = self.dscr("qsT", [NSH, 128, T], BF16, NSH)
        gfT = self.dscr("gfT", [NFH, 128, T], F32, NFH)
        hown = self.dscr("hown", [T, D], F32, NQ)
        oT = self.dscr("oT", [32, 128, T], BF16, 32)
        y1 = self.dscr("y1", [T, D], F32, NQ)
        h2s = self.dscr("h2s", [T, D], F32, NQ)
        self.h2chk = self.dscr("h2chk", [T, D], F32, NQ)
        self.h2fin = self.dscr("h2fin", [T, D], F32, NQ)

        pes = ExitStack()
        self.es.enter_context(pes)
        ident_f = self.sb(pes, "ident_f", [128, 128], F32)
        ident_b = self.sb(pes, "ident_b", [128, 128], BF16)
        ones_b = self.sb(pes, "ones_b", [128, 128], BF16)
        ones_f = self.sb(pes, "ones_f", [128, 128], F32)
        tri_f = self.sb(pes, "tri_f", [128, 128], F32)
        uincl_f = self.sb(pes, "uincl_f", [128, 128], F32)
        jones = self.sb(pes, "jones", [128, 128], F32)
        sel96 = self.sb(pes, "sel96", [96, NFH * 128], BF16)
        mbn = self.sb(pes, "mbn", [128, 3, 128], BF16)
        mbp = self.sb(pes, "mbp", [128, 3, 128], BF16)
        m01 = self.sb(pes, "m01", [128, 3, 128], F32)
        negc = self.sb(pes, "negc", [128, NB, NFH], F32)
        lfseq = self.sb(pes, "lfseq", [128, NB, NFH], F32)
        cT3 = self.sb(pes, "cT3", [96, T], BF16)
        psum = [Tn(pes.enter_context(nc.psum_tensor("ps%d" % i, [128, 512], F32))) for i in range(8)]
        for b_ in psum:
            b_.regs[0].excl = True
        self.psum = psum
        for dst, src in ((ident_f, c_ident_f), (ident_b, c_ident_b), (ones_b, c_ones_b), (ones_f, c_ones_f),
                         (tri_f, c_tri_f), (uincl_f, c_uincl_f), (jones, c_jones), (sel96, c_sel96),
                         (mbn, c_mbn), (mbp, c_mbp), (m01, c_m01)):
            self.load("sp", dst.t[:], src.ap(), [], [dst.R])
        self.consts = dict(ident_f=ident_f, ident_b=ident_b, ones_b=ones_b, ones_f=ones_f, tri_f=tri_f,
                           uincl_f=uincl_f, jones=jones, sel96=sel96, mbn=mbn, mbp=mbp, m01=m01)

        self._eps = {}
        for v in (LN_EPS, RMS_EPS, 1.0):
            t = self.sb(pes, "eps%g" % v, [128, 1], F32)
            P.op("dve", lambda e, t=t, v=v: e.memset(t.t[:], v), writes=[t.R])
            self._eps["eps%g" % v] = t
        w_in_v = w_in.ap().rearrange("(k p) c -> p k c", p=128)

        def featvec(es_, name, src, n=KC):
            t = self.sb(es_, name, [128, n], F32)
            tmp = self.sb(es_, name + "_row", [n, 128], F32)
            ap = src.ap()
            if len(ap.shape) == 1:
                ap = ap.rearrange("(k p) -> k p", p=128)
            self.load("sp", tmp.t[:], ap, [], [tmp.R])
            bank = psum[7]
            P.op("pe", lambda e: e.matmul(bank.t[:, 0:n], lhsT=tmp.t[:], rhs=ident_f.t[0:n, 0:n], start=True, stop=True),
                 reads=[tmp.R, ident_f.R], writes=[bank.R])
            P.op("dve", lambda e: e.tensor_copy(out=t.t[:], in_=bank.t[:, 0:n]), reads=[bank.R], writes=[t.R])
            return t
        self.featvec = featvec

        if "A" in phases:
            self.phase_inproj(x_seq, x_own, ln_in_g, ln_in_b, w_in_v, b_f, fq_g, fk_g,
                              kfT, ksT, vfS, vsS, qfT, qsT, gfT, hown, negc, lfseq, cT3, featvec)
        if "C" in phases:
            self.phase_attn(kfT, ksT, vfS, vsS, qfT, qsT, gfT, negc, cT3, mix_g, oT)
        if "D" in phases:
            self.phase_outproj(oT, w_out, hown, y1)
        if "E" in phases:
            self.phase_xa_moe(y1, mem_in, ln_mix_g, ln_mix_b, mem_ln_g, mem_ln_b, xa_wq, xa_wkv, xa_wo, ln_xa_g, ln_xa_b,
                              router_w, router_b, w_up, b_up, w_down, b_down, ln_moe_g, ln_moe_b, h2s, out_t, out_regs,
                              featvec)
        allregs = list(out_regs)
        for tn in (kfT, ksT, vfS, vsS, qfT, qsT, gfT, hown, oT, y1, h2s, self.h2chk, self.h2fin):
            allregs += tn.regs
        P.wait_all("sp", allregs)
        P.emit()
        return nc

    def ln_rows(self, es_, name):
        d = dict(
            st=self.sb(es_, name + "_st", [128, 8, 6], F32),
            mv=self.sb(es_, name + "_mv", [128, 2], F32),
            sd=self.sb(es_, name + "_sd", [128, 1], F32),
            rs=self.sb(es_, name + "_rs", [128, 1], F32),
            nm=self.sb(es_, name + "_nm", [128, 1], F32),
        )
        return d

    def ln_tile(self, xt, tmp, eps=LN_EPS, gbc=None, bbc=None):
        P = self.P
        st, mv, sd, rs, nm = tmp["st"], tmp["mv"], tmp["sd"], tmp["rs"], tmp["nm"]

        def f_stats(e):
            for c in range(8):
                i = e.bn_stats(out=st.t[:, c, :], in_=xt.t[:, c * 512:(c + 1) * 512])
            return i
        P.op("dve", f_stats, reads=[xt.R], writes=[st.R])
        P.op("dve", lambda e: e.bn_aggr(out=mv.t[:], in_=st.t[:].rearrange("p a b -> p (a b)")), reads=[st.R], writes=[mv.R])
        eps_ap = self.eps_tile(eps)
        P.op("act", lambda e: e.activation(out=sd.t[:], in_=mv.t[:, 1:2], func=AF.Sqrt, bias=eps_ap, scale=1.0),
             reads=[mv.R, self._eps["eps%g" % eps].R], writes=[sd.R])
        P.op("dve", lambda e: e.reciprocal(out=rs.t[:], in_=sd.t[:]), reads=[sd.R], writes=[rs.R])
        P.op("dve", lambda e: e.tensor_scalar(out=nm.t[:], in0=mv.t[:, 0:1], scalar1=rs.t[:, 0:1], scalar2=-1.0,
                                              op0=ALU.mult, op1=ALU.mult), reads=[mv.R, rs.R], writes=[nm.R])
        P.op("act", lambda e: e.activation(out=xt.t[:], in_=xt.t[:], func=AF.Identity, bias=nm.t[:, 0:1], scale=rs.t[:, 0:1]),
             reads=[xt.R, nm.R, rs.R], writes=[xt.R])
        if gbc is not None:
            P.op("dve", lambda e: e.tensor_tensor(out=xt.t[:], in0=xt.t[:], in1=gbc.t[:], op=ALU.mult), reads=[xt.R, gbc.R], writes=[xt.R])
            P.op("dve", lambda e: e.tensor_tensor(out=xt.t[:], in0=xt.t[:], in1=bbc.t[:], op=ALU.add), reads=[xt.R, bbc.R], writes=[xt.R])

    def eps_tile(self, eps):
        return self._eps["eps%g" % eps].t[:, 0:1]

    def transpose_tile(self, xt, dstT, dst_reg, t0, banks, gT=None, bT=None, dt_out=BF16, evac=("dve", "act")):
        P = self.P
        ident_f = self.consts["ident_f"]
        for g4 in range(8):
            bank = self.next_bank(banks)

            def f_tr(e, g4=g4, bank=bank):
                for i in range(4):
                    kc = g4 * 4 + i
                    ins = e.transpose(out=bank.t[:, i * 128:(i + 1) * 128], in_=xt.t[:, kc * 128:(kc + 1) * 128], identity=ident_f.t[:])
                return ins
            P.op("pe", f_tr, reads=[xt.R, ident_f.R], writes=[bank.R])
            eng = evac[g4 % len(evac)]
            if gT is None:
                src = bank.t[:].rearrange("p (a b) -> p a b", a=4)
                dst = dstT.t[:, g4 * 4:(g4 + 1) * 4, t0:t0 + 128]
                if eng == "act":
                    P.op("act", lambda e, s=src, d=dst: e.activation(out=d, in_=s, func=AF.Copy), reads=[bank.R], writes=[dst_reg])
                else:
                    P.op("dve", lambda e, s=src, d=dst: e.tensor_copy(out=d, in_=s), reads=[bank.R], writes=[dst_reg])
            else:
                def f_ev(e, g4=g4, bank=bank, eng=eng):
                    for i in range(4):
                        kc = g4 * 4 + i
                        d = dstT.t[:, kc, t0:t0 + 128]
                        s = bank.t[:, i * 128:(i + 1) * 128]
                        if eng == "act":
                            ins = e.activation(out=d, in_=s, func=AF.Identity, bias=bT.t[:, kc:kc + 1], scale=gT.t[:, kc:kc + 1])
                        else:
                            ins = e.tensor_scalar(out=d, in0=s, scalar1=gT.t[:, kc:kc + 1], scalar2=bT.t[:, kc:kc + 1],
                                                  op0=ALU.mult, op1=ALU.add)
                    return ins
                P.op(eng, f_ev, reads=[bank.R, gT.R, bT.R], writes=[dst_reg])

    def load_w(self, wb, src_view, col0, ncols, kc0=0, kc1=KC):
        mid = (kc0 + kc1) // 2
        for a, b in ((kc0, mid), (mid, kc1)):
            if b > a:
                self.load("pool", wb.t[:, a:b, 0:ncols], src_view[:, a:b, col0:col0 + ncols], [], [wb.R], max_dma_last_dim=4096)

    def gemm_fm(self, wbufs, src_view, col0, nheads, xT, xregs, ntok, banks, epilogue, nk=KC, pre=None):
        P = self.P
        wb = wbufs[self._wrr % len(wbufs)]
        self._wrr += 1
        self.load_w(wb, src_view, col0, nheads * 128, 0, nk)
        for hh in range(nheads):
            for (c0, c1) in _ceil_chunks(0, ntok, 512):
                bank = self.next_bank(banks)

                def f_mm(e, hh=hh, c0=c0, c1=c1, bank=bank, wb=wb):
                    for kc in range(nk):
                        ins = e.matmul(bank.t[:, 0:c1 - c0], lhsT=wb.t[:, kc, hh * 128:(hh + 1) * 128], rhs=xT.t[:, kc, c0:c1],
                                       start=(kc == 0), stop=(kc == nk - 1))
                    return ins
                P.op("pe", f_mm, reads=[wb.R] + [xregs[i] for i in range(c0 // 128, (c1 + 127) // 128)], writes=[bank.R])
                epilogue(hh, c0, c1, bank)

    def gemm_tm(self, wbufs, src_view, col0, ncols, xT, xregs, ntiles, banks, epilogue, nk=KC):
        P = self.P
        wb = wbufs[self._wrr % len(wbufs)]
        self._wrr += 1
        self.load_w(wb, src_view, col0, ncols, 0, nk)
        for tt in range(ntiles):
            bank = self.next_bank(banks)

            def f_mm(e, tt=tt, bank=bank, wb=wb):
                for kc in range(nk):
                    ins = e.matmul(bank.t[:, 0:ncols], lhsT=xT.t[:, kc, tt * 128:(tt + 1) * 128], rhs=wb.t[:, kc, 0:ncols],
                                   start=(kc == 0), stop=(kc == nk - 1))
                return ins
            P.op("pe", f_mm, reads=[wb.R, xregs[tt]], writes=[bank.R])
            epilogue(tt, bank)

    def phase_inproj(self, x_seq, x_own, ln_g, ln_b, w_in_v, b_f, fq_g, fk_g, kfT, ksT, vfS, vsS, qfT, qsT, gfT, hown,
                     negc, lfseq, cT3, featvec):
        self._wrr = 0
        for side in ("seq", "own"):
            self.inproj_side(side, x_seq, x_own, ln_g, ln_b, w_in_v, b_f, fq_g, fk_g, kfT, ksT, vfS, vsS, qfT, qsT, gfT, hown,
                             negc, lfseq, cT3, featvec)

    def inproj_side(self, side, x_seq, x_own, ln_g, ln_b, w_in_v, b_f, fq_g, fk_g, kfT, ksT, vfS, vsS, qfT, qsT, gfT, hown,
                    negc, lfseq, cT3, featvec):
        nc, P = self.nc, self.P
        C = self.consts
        psum = self.psum
        if True:
            ntiles = NB if side == "seq" else NQ
            ntok = ntiles * 128
            xsrc = x_seq if side == "seq" else x_own
            with self.scope() as es1:
                hT = self.sb(es1, "hT_" + side, [128, KC, ntok], BF16, ntiles)
                with self.scope() as es2:
                    xts = [self.sb(es2, "xt%d" % i, [128, D], F32) for i in range(2)]
                    tmp = self.ln_rows(es2, "lnA")
                    if side == "seq":
                        gT = featvec(es2, "gT_in", ln_g)
                        bT = featvec(es2, "bT_in", ln_b)
                        gbc = bbc = None
                    else:
                        gT = bT = None
                        gbc = self.sb(es2, "gbc", [128, D], F32)
                        bbc = self.sb(es2, "bbc", [128, D], F32)
                        self.load("sp", gbc.t[:], ln_g.ap().partition_broadcast(128), [], [gbc.R])
                        self.load("sp", bbc.t[:], ln_b.ap().partition_broadcast(128), [], [bbc.R])
                    for tt in range(ntiles):
                        xt = xts[tt % 2]
                        self.load("sp", xt.t[:], xsrc.ap()[tt * 128:(tt + 1) * 128, :], [], [xt.R])
                        self.ln_tile(xt, tmp, LN_EPS, gbc, bbc)
                        if side == "own":
                            self.load("sp", hown.t.ap()[tt * 128:(tt + 1) * 128, :], xt.t[:], [xt.R], [hown.regs[tt]])
                        self.transpose_tile(xt, hT, hT.regs[tt], tt * 128, psum[0:4], gT, bT)
                with self.scope() as es2:
                    wbufs = [self.sb(es2, "wb%d" % i, [128, KC, 256], BF16) for i in range(2)]
                    stage = [self.sb(es2, "stg%d" % i, [128, ntok], BF16) for i in range(2)]
                    stagef = [self.sb(es2, "stgf%d" % i, [128, T], F32) for i in range(2)] if side == "own" else None
                    vstage = [self.sb(es2, "vstg%d" % i, [128, NB, 256], BF16) for i in range(2)] if side == "seq" else None
                    sqb = [self.sb(es2, "sqb%d" % i, [128, 512], BF16) for i in range(2)]
                    lnv = [self.sb(es2, "lnv%d" % i, [128, 512], F32) for i in range(2)]
                    gk = featvec(es2, "gk", fk_g, 1)
                    gq = featvec(es2, "gq", fq_g, 1)
                    P.op("dve", lambda e: e.tensor_scalar(out=gq.t[:], in0=gq.t[:], scalar1=SCALE, scalar2=None, op0=ALU.mult),
                         reads=[gq.R], writes=[gq.R])
                    bfb = self.sb(es2, "bfb", [128, NFH], F32)
                    self.load("sp", bfb.t[:], b_f.ap().partition_broadcast(128), [], [bfb.R])
                    mm_banks = psum[0:4]
                    aux_banks = psum[4:6]
                    self._rr = 0
                    eps_rms = self.eps_tile(RMS_EPS)
                    one_ap = self.eps_tile(1.0)

                    def rms_epi(dst_scr, head0, gvec):
                        def epi(hh, c0, c1, bank):
                            i = self._rr % 2
                            self._rr += 1
                            w = c1 - c0
                            st = stage[(head0 + hh) % 2]
                            P.op("act", lambda e: e.activation(out=sqb[i].t[:, 0:w], in_=bank.t[:, 0:w], func=AF.Square),
                                 reads=[bank.R], writes=[sqb[i].R])
                            b2 = self.next_bank(aux_banks)
                            P.op("pe", lambda e: e.matmul(b2.t[:, 0:w], lhsT=C["ones_b"].t[:], rhs=sqb[i].t[:, 0:w], start=True, stop=True),
                                 reads=[sqb[i].R, C["ones_b"].R], writes=[b2.R])
                            P.op("act", lambda e: e.activation(out=lnv[i].t[:, 0:w], in_=b2.t[:, 0:w], func=AF.Ln,
                                                               bias=eps_rms, scale=1.0 / HD),
                                 reads=[b2.R, self._eps["eps%g" % RMS_EPS].R], writes=[lnv[i].R])
                            P.op("act", lambda e: e.activation(out=lnv[i].t[:, 0:w], in_=lnv[i].t[:, 0:w], func=AF.Exp, scale=-0.5),
                                 reads=[lnv[i].R], writes=[lnv[i].R])
                            P.op("dve", lambda e: e.scalar_tensor_tensor(out=st.t[:, c0:c1], in0=bank.t[:, 0:w], scalar=gvec.t[:, 0:1],
                                                                         in1=lnv[i].t[:, 0:w], op0=ALU.mult, op1=ALU.mult),
                                 reads=[bank.R, lnv[i].R, gvec.R], writes=[st.R])
                            if c1 == ntok:
                                h = head0 + hh
                                self.load("sp", dst_scr.t.ap()[h], st.t[:, 0:ntok], [st.R], [dst_scr.regs[h]])
                        return epi

                    def copy_epi(dst_scr, head0, scale, eng):
                        def epi(hh, c0, c1, bank):
                            w = c1 - c0
                            st = stage[(head0 + hh) % 2]
                            if eng == "act":
                                P.op("act", lambda e: e.activation(out=st.t[:, c0:c1], in_=bank.t[:, 0:w], func=AF.Copy, scale=scale),
                                     reads=[bank.R], writes=[st.R])
                            else:
                                P.op("dve", lambda e: e.tensor_scalar(out=st.t[:, c0:c1], in0=bank.t[:, 0:w], scalar1=scale, scalar2=None,
                                                                      op0=ALU.mult), reads=[bank.R], writes=[st.R])
                            if c1 == ntok:
                                h = head0 + hh
                                self.load("sp", dst_scr.t.ap()[h], st.t[:, 0:ntok], [st.R], [dst_scr.regs[h]])
                        return epi

                    def sig_epi(dst_scr, head0):
                        def epi(hh, c0, c1, bank):
                            w = c1 - c0
                            st = stagef[(head0 + hh) % 2]
                            P.op("act", lambda e: e.activation(out=st.t[:, c0:c1], in_=bank.t[:, 0:w], func=AF.Sigmoid),
                                 reads=[bank.R], writes=[st.R])
                            if c1 == ntok:
                                h = head0 + hh
                                self.load("sp", dst_scr.t.ap()[h], st.t[:, 0:ntok], [st.R], [dst_scr.regs[h]])
                        return epi

                    def v_epi(dst_scr, chunk):
                        vs_ = vstage[chunk % 2]

                        def epi(tt, bank):
                            eng = "dve" if tt % 2 == 0 else "act"
                            if eng == "act":
                                P.op("act", lambda e: e.activation(out=vs_.t[:, tt, :], in_=bank.t[:, 0:256], func=AF.Copy),
                                     reads=[bank.R], writes=[vs_.R])
                            else:
                                P.op("dve", lambda e: e.tensor_copy(out=vs_.t[:, tt, :], in_=bank.t[:, 0:256]), reads=[bank.R], writes=[vs_.R])
                            if tt == NB - 1:
                                dv = dst_scr.t.ap().rearrange("(t p) c -> p t c", p=128)[:, :, chunk * 256:(chunk + 1) * 256]
                                self.load("sp", dv, vs_.t[:], [vs_.R], [dst_scr.regs[chunk]])
                        return epi

                    if side == "seq":
                        for pr in range(8):
                            self.gemm_fm(wbufs, w_in_v, KF + pr * 256, 2, hT, hT.regs, ntok, mm_banks, rms_epi(kfT, pr * 2, gk))
                        for pr in range(8):
                            self.gemm_fm(wbufs, w_in_v, KS + pr * 256, 2, hT, hT.regs, ntok, mm_banks,
                                         copy_epi(ksT, pr * 2, 1.0, "act" if pr % 2 else "dve"))
                        for ch in range(8):
                            self.gemm_tm(wbufs, w_in_v, VF + ch * 256, 256, hT, hT.regs, NB, mm_banks, v_epi(vfS, ch))
                        for ch in range(8):
                            self.gemm_tm(wbufs, w_in_v, VS + ch * 256, 256, hT, hT.regs, NB, mm_banks, v_epi(vsS, ch))
                    else:
                        for pr in range(8):
                            self.gemm_fm(wbufs, w_in_v, QF + pr * 256, 2, hT, hT.regs, ntok, mm_banks, rms_epi(qfT, pr * 2, gq))
                        for pr in range(8):
                            self.gemm_fm(wbufs, w_in_v, QS + pr * 256, 2, hT, hT.regs, ntok, mm_banks,
                                         copy_epi(qsT, pr * 2, SCALE, "act" if pr % 2 else "dve"))
                        for pr in range(8):
                            self.gemm_fm(wbufs, w_in_v, GF + pr * 256, 2, hT, hT.regs, ntok, mm_banks, sig_epi(gfT, pr * 2))

                    lfown = self.sb(es2, "lfown", [128, NQ, NFH], F32)
                    cown = self.sb(es2, "cown", [128, NQ, NFH], F32)
                    xf = self.sb(es2, "xf", [128, NFH], F32)
                    lf_dst = lfseq if side == "seq" else lfown

                    def f_epi(tt, bank):
                        P.op("dve", lambda e: e.tensor_tensor(out=xf.t[:], in0=bank.t[:, 0:NFH], in1=bfb.t[:], op=ALU.add),
                             reads=[bank.R, bfb.R], writes=[xf.R])
                        P.op("act", lambda e: e.activation(out=xf.t[:], in_=xf.t[:], func=AF.Exp, scale=-1.0), reads=[xf.R], writes=[xf.R])
                        P.op("act", lambda e: e.activation(out=xf.t[:], in_=xf.t[:], func=AF.Ln, bias=one_ap, scale=1.0),
                             reads=[xf.R, self._eps["eps%g" % 1.0].R], writes=[xf.R])
                        P.op("dve", lambda e: e.tensor_scalar(out=lf_dst.t[:, tt, :], in0=xf.t[:], scalar1=-1.0, scalar2=None, op0=ALU.mult),
                             reads=[xf.R], writes=[lf_dst.R])
                    self.gemm_tm(wbufs, w_in_v, FL, NFH, hT, hT.regs, ntiles, mm_banks, f_epi)
                    for tt in range(ntiles):
                        bank = self.next_bank(aux_banks)

                        def f_cs(e, tt=tt, bank=bank):
                            terms = [(C["tri_f"], lf_dst.t[:, tt, :])]
                            for t2 in range(tt):
                                terms.append((C["ones_f"], lf_dst.t[:, t2, :]))
                            if side == "own":
                                for t2 in range(8):
                                    terms.append((C["jones"], lfseq.t[:, t2, :]))
                            for n, (l, r) in enumerate(terms):
                                ins = e.matmul(bank.t[:, 0:NFH], lhsT=l.t[:], rhs=r, start=(n == 0), stop=(n == len(terms) - 1))
                            return ins
                        P.op("pe", f_cs, reads=[lf_dst.R, lfseq.R, C["tri_f"].R, C["ones_f"].R, C["jones"].R], writes=[bank.R])
                        if side == "seq":
                            P.op("dve", lambda e, tt=tt, bank=bank: e.tensor_scalar(out=negc.t[:, tt, :], in0=bank.t[:, 0:NFH], scalar1=-1.0,
                                                                                   scalar2=None, op0=ALU.mult), reads=[bank.R], writes=[negc.R])
                        else:
                            P.op("dve", lambda e, tt=tt, bank=bank: e.tensor_copy(out=cown.t[:, tt, :], in_=bank.t[:, 0:NFH]),
                                 reads=[bank.R], writes=[cown.R])
                    if side == "own":
                        cTf = self.sb(es2, "cTf", [NFH, T], F32)
                        r1 = self.sb(es2, "r1", [NFH, T], F32)
                        hi = self.sb(es2, "hi", [NFH, T], BF16)
                        P.op("dve", lambda e: e.memset(cT3.t[:], 0.0), writes=[cT3.R])
                        for tt in range(NQ):
                            bank = self.next_bank(aux_banks)
                            P.op("pe", lambda e, tt=tt, bank=bank: e.transpose(out=bank.t[0:NFH, 0:128], in_=cown.t[:, tt, :], identity=C["ident_f"].t[:]),
                                 reads=[cown.R, C["ident_f"].R], writes=[bank.R])
                            P.op("dve", lambda e, tt=tt, bank=bank: e.tensor_copy(out=cTf.t[:, tt * 128:(tt + 1) * 128], in_=bank.t[0:NFH, 0:128]),
                                 reads=[bank.R], writes=[cTf.R])
                        P.op("dve", lambda e: e.tensor_copy(out=cT3.t[0:NFH, :], in_=cTf.t[:]), reads=[cTf.R], writes=[cT3.R])
                        P.op("dve", lambda e: e.tensor_tensor(out=r1.t[:], in0=cTf.t[:], in1=cT3.t[0:NFH, :], op=ALU.subtract),
                             reads=[cTf.R, cT3.R], writes=[r1.R])
                        P.op("dve", lambda e: e.tensor_copy(out=cT3.t[32:32 + NFH, :], in_=r1.t[:]), reads=[r1.R], writes=[cT3.R])
                        P.op("dve", lambda e: e.tensor_copy(out=hi.t[:], in_=r1.t[:]), reads=[r1.R], writes=[hi.R])
                        P.op("dve", lambda e: e.tensor_tensor(out=r1.t[:], in0=r1.t[:], in1=hi.t[:], op=ALU.subtract),
                             reads=[r1.R, hi.R], writes=[r1.R])
                        P.op("dve", lambda e: e.tensor_copy(out=cT3.t[64:64 + NFH, :], in_=r1.t[:]), reads=[r1.R], writes=[cT3.R])

    @staticmethod
    def unit_type(s, kb):
        if s > kb:
            return None
        if s == kb:
            return 0
        if s == kb - 8:
            return 2
        return 1

    @staticmethod
    def att_chunks(kb):
        s0 = max(0, kb - 8)
        a0 = s0 * 128
        out = []
        if a0 < 512:
            out.append((a0, 512))
        out.append((max(a0, 512), 1024))
        return out

    def phase_attn(self, kfT, ksT, vfS, vsS, qfT, qsT, gfT, negc, cT3, mix_g, oT):
        nc, P = self.nc, self.P
        C = self.consts
        ps = self.psum
        with self.scope() as es1:
            kT = [self.sb(es1, "kT%d" % i, [128, S], BF16) for i in range(2)]
            qT = [self.sb(es1, "qT%d" % i, [128, T], BF16) for i in range(2)]
            qn = [self.sb(es1, "qn%d" % i, [128, T], BF16) for i in range(2)]
            vv = [self.sb(es1, "vv%d" % i, [128, NB, 128], BF16) for i in range(2)]
            gg = [self.sb(es1, "gg%d" % i, [128, T], F32) for i in range(2)]
            pt = [self.sb(es1, "pt%d" % i, [128, 512], BF16) for i in range(3)]
            et = [self.sb(es1, "et%d" % i, [128, 512], F32) for i in range(2)]
            spt = [self.sb(es1, "spt%d" % i, [128, 512], F32) for i in range(2)]
            Racc = self.sb(es1, "Racc", [128, T], F32, 2)
            oc = self.sb(es1, "oc", [128, T], F32, 2)
            denc = self.sb(es1, "denc", [128, T], F32, 2)
            sq = self.sb(es1, "sq", [128, T], BF16, 2)
            tot = self.sb(es1, "tot", [128, T], F32, 2)
            ostg = [self.sb(es1, "ostg%d" % i, [128, T], BF16) for i in range(2)]
            zer = self.sb(es1, "zer", [128, 512], BF16)
            mixgT = self.featvec(es1, "mixgT", mix_g, 32)
            P.op("dve", lambda e: e.memset(zer.t[:], 0.0), writes=[zer.R])
            eps_rms = self.eps_tile(RMS_EPS)
            one_ap = self.eps_tile(1.0)
            epsR = self._eps["eps%g" % RMS_EPS].R
            oneR = self._eps["eps%g" % 1.0].R
            OT = [ps[4], ps[5]]
            DEN = [ps[6], ps[7]]
            self._prr = 0

            def post(h_out, is_fox, slot):
                st = ostg[h_out % 2]
                for half in range(2):
                    c0, c1 = half * 512, (half + 1) * 512
                    R2 = [oc.regs[half]]
                    P.op("act", lambda e, c0=c0, c1=c1: e.activation(out=sq.t[:, c0:c1], in_=oc.t[:, c0:c1], func=AF.Square),
                         reads=R2, writes=[sq.regs[half]])
                    bank = ps[3]
                    P.op("pe", lambda e, c0=c0, c1=c1, bank=bank: e.matmul(bank.t[:, 0:512], lhsT=C["ones_b"].t[:], rhs=sq.t[:, c0:c1],
                                                                         start=True, stop=True),
                         reads=[sq.regs[half], C["ones_b"].R], writes=[bank.R])
                    if is_fox:
                        P.op("act", lambda e, c0=c0, c1=c1: e.activation(out=denc.t[:, c0:c1], in_=denc.t[:, c0:c1], func=AF.Square,
                                                                         scale=RMS_EPS ** 0.5),
                             reads=[denc.regs[half]], writes=[denc.regs[half]])
                        P.op("dve", lambda e, c0=c0, c1=c1, bank=bank: e.scalar_tensor_tensor(
                            out=tot.t[:, c0:c1], in0=bank.t[:, 0:512], scalar=1.0 / HD, in1=denc.t[:, c0:c1], op0=ALU.mult, op1=ALU.add),
                            reads=[bank.R, denc.regs[half]], writes=[tot.regs[half]])
                        P.op("act", lambda e, c0=c0, c1=c1: e.activation(out=tot.t[:, c0:c1], in_=tot.t[:, c0:c1], func=AF.Ln),
                             reads=[tot.regs[half]], writes=[tot.regs[half]])
                    else:
                        P.op("act", lambda e, c0=c0, c1=c1, bank=bank: e.activation(out=tot.t[:, c0:c1], in_=bank.t[:, 0:512], func=AF.Ln,
                                                                                    bias=eps_rms, scale=1.0 / HD),
                             reads=[bank.R, epsR], writes=[tot.regs[half]])
                    P.op("act", lambda e, c0=c0, c1=c1: e.activation(out=tot.t[:, c0:c1], in_=tot.t[:, c0:c1], func=AF.Exp, scale=-0.5),
                         reads=[tot.regs[half]], writes=[tot.regs[half]])
                    if is_fox:
                        P.op("dve", lambda e, c0=c0, c1=c1: e.scalar_tensor_tensor(
                            out=oc.t[:, c0:c1], in0=oc.t[:, c0:c1], scalar=mixgT.t[:, h_out:h_out + 1], in1=tot.t[:, c0:c1],
                            op0=ALU.mult, op1=ALU.mult), reads=[oc.regs[half], tot.regs[half], mixgT.R], writes=[oc.regs[half]])
                        P.op("dve", lambda e, c0=c0, c1=c1: e.tensor_tensor(out=st.t[:, c0:c1], in0=oc.t[:, c0:c1], in1=gg[slot].t[:, c0:c1],
                                                                            op=ALU.mult),
                             reads=[oc.regs[half], gg[slot].R], writes=[st.R])
                    else:
                        P.op("dve", lambda e, c0=c0, c1=c1: e.scalar_tensor_tensor(
                            out=st.t[:, c0:c1], in0=oc.t[:, c0:c1], scalar=mixgT.t[:, h_out:h_out + 1], in1=tot.t[:, c0:c1],
                            op0=ALU.mult, op1=ALU.mult), reads=[oc.regs[half], tot.regs[half], mixgT.R], writes=[st.R])
                self.load("sp", oT.t.ap()[h_out], st.t[:], [st.R], [oT.regs[h_out]])

            for hh in range(NFH + NSH):
                is_fox = hh < NFH
                h = hh if is_fox else hh - NFH
                sl = hh % 2
                k_scr, q_scr, v_scr = (kfT, qfT, vfS) if is_fox else (ksT, qsT, vsS)
                self.load("sp", kT[sl].t[:], k_scr.t.ap()[h], [k_scr.regs[h]], [kT[sl].R])
                self.load("sp", qT[sl].t[:], q_scr.t.ap()[h], [q_scr.regs[h]], [qT[sl].R])
                self.load("sp", vv[sl].t[:], v_scr.t.ap().rearrange("(b p) c -> p b c", p=128)[:, :, h * 128:(h + 1) * 128],
                          [v_scr.regs[h // 2]], [vv[sl].R])
                if is_fox:
                    self.load("sp", gg[sl].t[:], gfT.t.ap()[h], [gfT.regs[h]], [gg[sl].R])
                else:
                    P.op("pool", lambda e, sl=sl: e.tensor_scalar(out=qn[sl].t[:], in0=qT[sl].t[:], scalar1=-1.0, scalar2=None, op0=ALU.mult),
                         reads=[qT[sl].R], writes=[qn[sl].R])
                    P.op("pool", lambda e: e.memset(Racc.t[:], 0.0), writes=[Racc.regs[0], Racc.regs[1]])
                    for b in range(2):
                        P.op("pe", lambda e, b=b, sl=sl: e.matmul(OT[b].t[:, 0:512], lhsT=vv[sl].t[:, 0, :], rhs=zer.t[:, 0:512], start=True, stop=False,
                                                                  skip_group_check=True),
                             reads=[vv[sl].R, zer.R], writes=[OT[b].R])
                kbs = range(NB) if is_fox else range(NB - 1, -1, -1)
                for kb in kbs:
                    for (c0, c1) in self.att_chunks(kb):
                        w = c1 - c0
                        half = 0 if c0 < 512 else 1
                        base = half * 512
                        slots = range(c0 // 128, c1 // 128)
                        masked = [(s, self.unit_type(s, kb)) for s in slots if self.unit_type(s, kb) is not None]
                        if is_fox:
                            sb_ = ps[self._prr % 3]
                            self._prr += 1
                            pti = pt[self._prr % 3]

                            def f_s(e, kb=kb, c0=c0, c1=c1, w=w, sb_=sb_, sl=sl, h=h, masked=masked):
                                e.matmul(sb_.t[:, 0:w], lhsT=kT[sl].t[:, kb * 128:(kb + 1) * 128], rhs=qT[sl].t[:, c0:c1], start=True, stop=False,
                                         skip_group_check=True)
                                ins = e.matmul(sb_.t[:, 0:w], lhsT=C["sel96"].t[:, h * 128:(h + 1) * 128], rhs=cT3.t[:, c0:c1], start=False,
                                               stop=(len(masked) == 0), skip_group_check=True)
                                for n, (s, ty) in enumerate(masked):
                                    o = s * 128 - c0
                                    ins = e.matmul(sb_.t[:, o:o + 128], lhsT=C["ident_b"].t[:], rhs=C["mbn"].t[:, ty, :], start=False,
                                                   stop=(n == len(masked) - 1), skip_group_check=True)
                                return ins
                            P.op("pe", f_s, reads=[kT[sl].R, qT[sl].R, C["sel96"].R, cT3.R, C["ident_b"].R, C["mbn"].R], writes=[sb_.R])
                            P.op("act", lambda e, w=w, sb_=sb_, pti=pti, kb=kb, h=h: e.activation(
                                out=pti.t[:, 0:w], in_=sb_.t[:, 0:w], func=AF.Exp, bias=negc.t[:, kb, h:h + 1], scale=1.0),
                                reads=[sb_.R, negc.R], writes=[pti.R])

                            def f_pv(e, kb=kb, c0=c0, w=w, base=base, half=half, pti=pti, sl=sl):
                                e.matmul(OT[half].t[:, c0 - base:c0 - base + w], lhsT=vv[sl].t[:, kb, :], rhs=pti.t[:, 0:w], start=(kb == 0),
                                         stop=(kb == NB - 1), skip_group_check=True)
                                return e.matmul(DEN[half].t[:, c0 - base:c0 - base + w], lhsT=C["ones_b"].t[:], rhs=pti.t[:, 0:w], start=(kb == 0),
                                                stop=(kb == NB - 1), skip_group_check=True)
                            P.op("pe", f_pv, reads=[pti.R, vv[sl].R, C["ones_b"].R], writes=[OT[half].R, DEN[half].R])
                        else:
                            zb = ps[self._prr % 2]
                            tb = ps[2 + (self._prr % 2)]
                            eti = et[self._prr % 2]
                            spi = spt[self._prr % 2]
                            ati = pt[self._prr % 3]
                            self._prr += 1
                            P.op("pe", lambda e, kb=kb, c0=c0, c1=c1, w=w, zb=zb, sl=sl: e.matmul(
                                zb.t[:, 0:w], lhsT=kT[sl].t[:, kb * 128:(kb + 1) * 128], rhs=qT[sl].t[:, c0:c1], start=True, stop=True),
                                reads=[kT[sl].R, qT[sl].R], writes=[zb.R])
                            P.op("act", lambda e, w=w, zb=zb, eti=eti: e.activation(out=eti.t[:, 0:w], in_=zb.t[:, 0:w], func=AF.Exp),
                                 reads=[zb.R], writes=[eti.R])
                            P.op("act", lambda e, w=w, eti=eti, spi=spi: e.activation(out=spi.t[:, 0:w], in_=eti.t[:, 0:w], func=AF.Ln, bias=one_ap,
                                                                                      scale=1.0),
                                 reads=[eti.R, oneR], writes=[spi.R])
                            for (s, ty) in masked:
                                o = s * 128 - c0
                                P.op("dve", lambda e, o=o, ty=ty, spi=spi: e.tensor_tensor(out=spi.t[:, o:o + 128], in0=spi.t[:, o:o + 128],
                                                                                           in1=C["m01"].t[:, ty, :], op=ALU.mult),
                                     reads=[spi.R, C["m01"].R], writes=[spi.R])

                            def f_t(e, kb=kb, c0=c0, c1=c1, w=w, tb=tb, spi=spi, sl=sl, masked=masked):
                                e.matmul(tb.t[:, 0:w], lhsT=C["uincl_f"].t[:], rhs=spi.t[:, 0:w], start=True, stop=False, skip_group_check=True)
                                e.matmul(tb.t[:, 0:w], lhsT=C["ones_f"].t[:], rhs=Racc.t[:, c0:c1], start=False, stop=False, skip_group_check=True)
                                ins = e.matmul(tb.t[:, 0:w], lhsT=kT[sl].t[:, kb * 128:(kb + 1) * 128], rhs=qn[sl].t[:, c0:c1], start=False,
                                               stop=(len(masked) == 0), skip_group_check=True)
                                for n, (s, ty) in enumerate(masked):
                                    o = s * 128 - c0
                                    ins = e.matmul(tb.t[:, o:o + 128], lhsT=C["ident_b"].t[:], rhs=C["mbp"].t[:, ty, :], start=False,
                                                   stop=(n == len(masked) - 1), skip_group_check=True)
                                return ins
                            P.op("pe", f_t, reads=[spi.R, Racc.regs[half], kT[sl].R, qn[sl].R, C["uincl_f"].R, C["ones_f"].R, C["ident_b"].R,
                                                   C["mbp"].R], writes=[tb.R])
                            P.op("dve", lambda e, c0=c0, c1=c1, w=w, spi=spi: e.tensor_tensor(out=Racc.t[:, c0:c1], in0=Racc.t[:, c0:c1],
                                                                                               in1=spi.t[:, 0:w], op=ALU.add),
                                 reads=[spi.R, Racc.regs[half]], writes=[Racc.regs[half]])
                            P.op("act", lambda e, w=w, tb=tb, ati=ati: e.activation(out=ati.t[:, 0:w], in_=tb.t[:, 0:w], func=AF.Exp, scale=-1.0),
                                 reads=[tb.R], writes=[ati.R])
                            P.op("pe", lambda e, kb=kb, c0=c0, w=w, base=base, half=half, ati=ati, sl=sl: e.matmul(
                                OT[half].t[:, c0 - base:c0 - base + w], lhsT=vv[sl].t[:, kb, :], rhs=ati.t[:, 0:w], start=False, stop=(kb == 0),
                                skip_group_check=True), reads=[ati.R, vv[sl].R], writes=[OT[half].R])
                for half in range(2):
                    c0, c1 = half * 512, (half + 1) * 512
                    P.op("dve", lambda e, c0=c0, c1=c1, half=half: e.tensor_copy(out=oc.t[:, c0:c1], in_=OT[half].t[:, 0:512]),
                         reads=[OT[half].R], writes=[oc.regs[half]])
                    if is_fox:
                        P.op("act", lambda e, c0=c0, c1=c1, half=half: e.activation(out=denc.t[:, c0:c1], in_=DEN[half].t[:, 0:512], func=AF.Copy),
                             reads=[DEN[half].R], writes=[denc.regs[half]])
                post(hh, is_fox, sl)

    def phase_outproj(self, oT, w_out, hown, y1):
        nc, P = self.nc, self.P
        ps = self.psum
        w_v = w_out.ap().rearrange("(k p) c -> p k c", p=128)
        with self.scope() as es1:
            oTs = self.sb(es1, "oTs", [128, KC, T], BF16, KC)
            wb = [self.sb(es1, "wo%d" % i, [128, KC, 512], BF16) for i in range(2)]
            hp = [self.sb(es1, "hp%d" % i, [128, 512], F32) for i in range(3)]
            ys = [self.sb(es1, "ys%d" % i, [128, 512], F32) for i in range(3)]
            for k in range(KC):
                self.load("sp", oTs.t[:, k, :], oT.t.ap()[k], [oT.regs[k]], [oTs.regs[k]])
            n = 0
            for ch in range(8):
                w = wb[ch % 2]
                for a, b in ((0, 16), (16, 32)):
                    self.load("pool", w.t[:, a:b, :], w_v[:, a:b, ch * 512:(ch + 1) * 512], [], [w.R], max_dma_last_dim=4096)
                for tt in range(NQ):
                    bank = ps[n % 4]
                    hpi, ysi = hp[n % 3], ys[n % 3]
                    n += 1
                    self.load("sp", hpi.t[:], hown.t.ap()[tt * 128:(tt + 1) * 128, ch * 512:(ch + 1) * 512], [hown.regs[tt]], [hpi.R])

                    def f_mm(e, tt=tt, bank=bank, w=w):
                        for kc in range(KC):
                            ins = e.matmul(bank.t[:, 0:512], lhsT=oTs.t[:, kc, tt * 128:(tt + 1) * 128], rhs=w.t[:, kc, :],
                                           start=(kc == 0), stop=(kc == KC - 1))
                        return ins
                    P.op("pe", f_mm, reads=[w.R] + oTs.regs, writes=[bank.R])
                    P.op("dve", lambda e, bank=bank, hpi=hpi, ysi=ysi: e.scalar_tensor_tensor(
                        out=ysi.t[:], in0=hpi.t[:], scalar=ALPHA, in1=bank.t[:, 0:512], op0=ALU.mult, op1=ALU.add),
                        reads=[bank.R, hpi.R], writes=[ysi.R])
                    self.load("sp", y1.t.ap()[tt * 128:(tt + 1) * 128, ch * 512:(ch + 1) * 512], ysi.t[:], [ysi.R], [y1.regs[tt]])

    def phase_xa_moe(self, y1, mem_in, ln_mix_g, ln_mix_b, mem_ln_g, mem_ln_b, xa_wq, xa_wkv, xa_wo, ln_xa_g, ln_xa_b,
                     router_w, router_b, w_up, b_up, w_down, b_down, ln_moe_g, ln_moe_b, h2s, out_t, out_regs, featvec):
        nc, P = self.nc, self.P
        C = self.consts
        ps = self.psum
        NEX = self.debug.get("ne", NE)
        y2s = h2s
        with self.scope() as es1:
            gbc = self.sb(es1, "gbc1", [128, D], F32)
            bbc = self.sb(es1, "bbc1", [128, D], F32)
            self.load("sp", gbc.t[:], ln_mix_g.ap().partition_broadcast(128), [], [gbc.R])
            self.load("sp", bbc.t[:], ln_mix_b.ap().partition_broadcast(128), [], [bbc.R])
            wq = self.sb(es1, "wq", [128, KC, XAW], BF16)
            wo = self.sb(es1, "wo", [128, 4, D], BF16)
            kx = self.sb(es1, "kx", [128, 4, NMEM], BF16)
            vx = self.sb(es1, "vx", [128, 2, XAW], BF16)
            wq_v = xa_wq.ap().rearrange("(k p) c -> p k c", p=128)
            wkv_v = xa_wkv.ap().rearrange("(k p) c -> p k c", p=128)
            wo_v = xa_wo.ap().rearrange("(k p) c -> p k c", p=128)
            for a, b in ((0, 16), (16, 32)):
                self.load("pool", wq.t[:, a:b, :], wq_v[:, a:b, :], [], [wq.R], max_dma_last_dim=4096)
            for a, b in ((0, 2), (2, 4)):
                for c in range(4):
                    self.load("pool", wo.t[:, a:b, c * 1024:(c + 1) * 1024], wo_v[:, a:b, c * 1024:(c + 1) * 1024], [], [wo.R], max_dma_last_dim=4096)
            tmp = self.ln_rows(es1, "lnE")
            with self.scope() as es2:
                memT = self.sb(es2, "memT", [128, KC, NMEM], BF16, 2)
                gT = featvec(es2, "gT_mem", mem_ln_g)
                bT = featvec(es2, "bT_mem", mem_ln_b)
                xm = self.sb(es2, "xm", [128, D], F32)
                wkv = self.sb(es2, "wkv", [128, KC, 512], BF16)
                for tt in range(2):
                    self.load("sp", xm.t[:], mem_in.ap()[tt * 128:(tt + 1) * 128, :], [], [xm.R])
                    self.ln_tile(xm, tmp, LN_EPS)
                    self.transpose_tile(xm, memT, memT.regs[tt], tt * 128, ps[0:4], gT, bT)
                for a, b in ((0, 16), (16, 32)):
                    self.load("pool", wkv.t[:, a:b, :], wkv_v[:, a:b, 0:512], [], [wkv.R], max_dma_last_dim=4096)
                for hx in range(4):
                    bank = ps[hx % 4]

                    def f_k(e, hx=hx, bank=bank):
                        for kc in range(KC):
                            ins = e.matmul(bank.t[:, 0:NMEM], lhsT=wkv.t[:, kc, hx * 128:(hx + 1) * 128], rhs=memT.t[:, kc, :],
                                           start=(kc == 0), stop=(kc == KC - 1))
                        return ins
                    P.op("pe", f_k, reads=[wkv.R] + memT.regs, writes=[bank.R])
                    P.op("dve", lambda e, hx=hx, bank=bank: e.tensor_copy(out=kx.t[:, hx, :], in_=bank.t[:, 0:NMEM]), reads=[bank.R], writes=[kx.R])
                for a, b in ((0, 16), (16, 32)):
                    self.load("pool", wkv.t[:, a:b, :], wkv_v[:, a:b, 512:1024], [], [wkv.R], max_dma_last_dim=4096)
                for mt in range(2):
                    bank = ps[mt % 4]

                    def f_v(e, mt=mt, bank=bank):
                        for kc in range(KC):
                            ins = e.matmul(bank.t[:, 0:512], lhsT=memT.t[:, kc, mt * 128:(mt + 1) * 128], rhs=wkv.t[:, kc, :],
                                           start=(kc == 0), stop=(kc == KC - 1))
                        return ins
                    P.op("pe", f_v, reads=[wkv.R] + memT.regs, writes=[bank.R])
                    P.op("dve", lambda e, mt=mt, bank=bank: e.tensor_copy(out=vx.t[:, mt, :], in_=bank.t[:, 0:512]), reads=[bank.R], writes=[vx.R])
            with self.scope() as es2:
                xt = self.sb(es2, "xtE", [128, D], F32)
                y2t = self.sb(es2, "y2t", [128, D], F32)
                h1T = self.sb(es2, "h1T", [128, KC, 128], BF16)
                qx = self.sb(es2, "qx", [128, 4, 128], BF16)
                pT = self.sb(es2, "pT", [128, 2, 512], BF16)
                rec = self.sb(es2, "rec", [128, 512], F32)
                xo = self.sb(es2, "xo", [128, 4, 128], BF16)
                for tt in range(NQ):
                    self.load("sp", xt.t[:], y1.t.ap()[tt * 128:(tt + 1) * 128, :], [y1.regs[tt]], [xt.R])
                    self.ln_tile(xt, tmp, LN_EPS, gbc, bbc)
                    self.transpose_tile(xt, h1T, h1T.R, 0, ps[0:2])
                    qb = ps[2]

                    def f_q(e, qb=qb):
                        for hx in range(4):
                            for kc in range(KC):
                                ins = e.matmul(qb.t[:, hx * 128:(hx + 1) * 128], lhsT=wq.t[:, kc, hx * 128:(hx + 1) * 128], rhs=h1T.t[:, kc, :],
                                               start=(kc == 0), stop=(kc == KC - 1), skip_group_check=True)
                        return ins
                    P.op("pe", f_q, reads=[wq.R, h1T.R], writes=[qb.R])
                    P.op("act", lambda e, qb=qb: e.activation(out=qx.t[:].rearrange("p a b -> p (a b)"), in_=qb.t[:, 0:512], func=AF.Copy, scale=SCALE),
                         reads=[qb.R], writes=[qx.R])
                    for mt in range(2):
                        sbk = ps[3 + mt]

                        def f_s(e, mt=mt, sbk=sbk):
                            for hx in range(4):
                                ins = e.matmul(sbk.t[:, hx * 128:(hx + 1) * 128], lhsT=kx.t[:, hx, mt * 128:(mt + 1) * 128], rhs=qx.t[:, hx, :],
                                               start=True, stop=True, skip_group_check=True)
                            return ins
                        P.op("pe", f_s, reads=[kx.R, qx.R], writes=[sbk.R])
                        P.op("act", lambda e, mt=mt, sbk=sbk: e.activation(out=pT.t[:, mt, :], in_=sbk.t[:, 0:512], func=AF.Exp), reads=[sbk.R], writes=[pT.R])
                    ob, db = ps[5], ps[6]

                    def f_o(e, ob=ob, db=db):
                        for hx in range(4):
                            for mt in range(2):
                                e.matmul(ob.t[:, hx * 128:(hx + 1) * 128], lhsT=vx.t[:, mt, hx * 128:(hx + 1) * 128], rhs=pT.t[:, mt, hx * 128:(hx + 1) * 128],
                                         start=(mt == 0), stop=(mt == 1), skip_group_check=True)
                        for mt in range(2):
                            ins = e.matmul(db.t[:, 0:512], lhsT=C["ones_b"].t[:], rhs=pT.t[:, mt, :], start=(mt == 0), stop=(mt == 1))
                        return ins
                    P.op("pe", f_o, reads=[vx.R, pT.R, C["ones_b"].R], writes=[ob.R, db.R])
                    P.op("dve", lambda e, db=db: e.reciprocal(out=rec.t[:], in_=db.t[:, 0:512]), reads=[db.R], writes=[rec.R])
                    P.op("dve", lambda e, ob=ob: e.tensor_tensor(out=xo.t[:].rearrange("p a b -> p (a b)"), in0=ob.t[:, 0:512], in1=rec.t[:], op=ALU.mult),
                         reads=[ob.R, rec.R], writes=[xo.R])
                    for ch in range(8):
                        bank = ps[ch % 2] if ch % 2 == 0 else ps[7]

                        def f_w(e, ch=ch, bank=bank):
                            for hx in range(4):
                                ins = e.matmul(bank.t[:, 0:512], lhsT=xo.t[:, hx, :], rhs=wo.t[:, hx, ch * 512:(ch + 1) * 512], start=(hx == 0), stop=(hx == 3))
                            return ins
                        P.op("pe", f_w, reads=[xo.R, wo.R], writes=[bank.R])
                        P.op("dve", lambda e, ch=ch, bank=bank: e.scalar_tensor_tensor(
                            out=y2t.t[:, ch * 512:(ch + 1) * 512], in0=xt.t[:, ch * 512:(ch + 1) * 512], scalar=ALPHA, in1=bank.t[:, 0:512],
                            op0=ALU.mult, op1=ALU.add), reads=[bank.R, xt.R], writes=[y2t.R])
                    self.load("sp", y2s.t.ap()[tt * 128:(tt + 1) * 128, :], y2t.t[:], [y2t.R], [y2s.regs[tt]])
        if self.debug.get("stop") == "E1":
            return
        for half in range(2):
            self.moe_half(half, NEX, y2s, ln_xa_g, ln_xa_b, router_w, router_b, w_up, b_up, w_down, b_down, ln_moe_g, ln_moe_b, out_t, out_regs)

    def moe_half(self, half, NEX, y2s, ln_xa_g, ln_xa_b, router_w, router_b, w_up, b_up, w_down, b_down, ln_moe_g, ln_moe_b, out_t, out_regs):
        nc, P = self.nc, self.P
        C = self.consts
        ps = self.psum
        NT = 4
        NTOK = NT * 128
        with self.scope() as es1:
            acc = self.sb(es1, "acc", [128, NT, D], F32, NT)
            h2T = self.sb(es1, "h2T", [128, KC, NTOK], BF16, NT)
            gate = self.sb(es1, "gate", [128, NT, NE], F32, NT)
            bupT = self.sb(es1, "bupT", [128, 16, NE], F32)
            with self.scope() as es2:
                gbc = self.sb(es2, "gbc2", [128, D], F32)
                bbc = self.sb(es2, "bbc2", [128, D], F32)
                self.load("sp", gbc.t[:], ln_xa_g.ap().partition_broadcast(128), [], [gbc.R])
                self.load("sp", bbc.t[:], ln_xa_b.ap().partition_broadcast(128), [], [bbc.R])
                tmp = self.ln_rows(es2, "lnF")
                xt = self.sb(es2, "xtF", [128, D], F32)
                h2Tf = self.sb(es2, "h2Tf", [128, KC, 128], F32)
                rw = self.sb(es2, "rw", [128, KC, NE], F32)
                rbb = self.sb(es2, "rbb", [128, NE], F32)
                lg = self.sb(es2, "lg", [128, NE], F32)
                top8 = self.sb(es2, "top8", [128, 8], F32)
                nmx = self.sb(es2, "nmx", [128, 1], F32)
                msk = self.sb(es2, "msk", [128, NE], F32)
                ex = self.sb(es2, "ex", [128, NE], F32)
                ssum = self.sb(es2, "ssum", [128, 1], F32)
                gTs = self.sb(es2, "gTs", [NE, NTOK], F32)
                bdn = self.sb(es2, "bdn", [NE, D], F32)
                bup = self.sb(es2, "bup", [NE, 2 * FE], F32)
                with nc.allow_non_contiguous_dma(reason="small router weight load"):
                    self.load("sp", rw.t[:], router_w.ap().rearrange("(k p) c -> p k c", p=128), [], [rw.R], allow_slow_non_contiguous=True)
                self.load("sp", rbb.t[:], router_b.ap().partition_broadcast(128), [], [rbb.R])
                self.load("sp", bdn.t[:], b_down.ap(), [], [bdn.R])
                self.load("sp", bup.t[:], b_up.ap(), [], [bup.R])
                for c in range(16):
                    bank = ps[c % 2]
                    P.op("pe", lambda e, c=c, bank=bank: e.transpose(out=bank.t[:, 0:NE], in_=bup.t[:, c * 128:(c + 1) * 128], identity=C["ident_f"].t[0:NE, 0:NE]),
                         reads=[bup.R, C["ident_f"].R], writes=[bank.R])
                    P.op("dve", lambda e, c=c, bank=bank: e.tensor_copy(out=bupT.t[:, c, :], in_=bank.t[:, 0:NE]), reads=[bank.R], writes=[bupT.R])
                if self.debug.get("stop") == "E2a":
                    return
                for tl in range(NT):
                    tt = half * NT + tl
                    self.load("sp", xt.t[:], y2s.t.ap()[tt * 128:(tt + 1) * 128, :], [y2s.regs[tt]], [xt.R])
                    self.load("sp", self.h2chk.t.ap()[tt * 128:(tt + 1) * 128, :], xt.t[:], [xt.R], [self.h2chk.regs[tt]])
                    self.ln_tile(xt, tmp, LN_EPS, gbc, bbc)
                    self.load("sp", self.h2fin.t.ap()[tt * 128:(tt + 1) * 128, :], xt.t[:], [xt.R], [self.h2fin.regs[tt]])
                    for g4 in range(8):
                        bank = ps[g4 % 4]

                        def f_tr(e, g4=g4, bank=bank):
                            for i in range(4):
                                kc = g4 * 4 + i
                                ins = e.transpose(out=bank.t[:, i * 128:(i + 1) * 128], in_=xt.t[:, kc * 128:(kc + 1) * 128], identity=C["ident_f"].t[:])
                            return ins
                        P.op("pe", f_tr, reads=[xt.R, C["ident_f"].R], writes=[bank.R])
                        src = bank.t[:].rearrange("p (a b) -> p a b", a=4)
                        P.op("act", lambda e, g4=g4, src=src: e.activation(out=h2Tf.t[:, g4 * 4:(g4 + 1) * 4, :], in_=src, func=AF.Copy),
                             reads=[bank.R], writes=[h2Tf.R])
                        P.op("dve", lambda e, g4=g4, tl=tl: e.tensor_copy(out=h2T.t[:, g4 * 4:(g4 + 1) * 4, tl * 128:(tl + 1) * 128],
                                                                         in_=h2Tf.t[:, g4 * 4:(g4 + 1) * 4, :]),
                             reads=[h2Tf.R], writes=[h2T.regs[tl]])
                    P.op("act", lambda e, tl=tl: e.activation(out=acc.t[:, tl, :], in_=xt.t[:], func=AF.Copy, scale=ALPHA), reads=[xt.R], writes=[acc.regs[tl]])
                    lb = ps[4]

                    def f_r(e, lb=lb):
                        for kc in range(KC):
                            ins = e.matmul(lb.t[:, 0:NE], lhsT=h2Tf.t[:, kc, :], rhs=rw.t[:, kc, :], start=(kc == 0), stop=(kc == KC - 1))
                        return ins
                    P.op("pe", f_r, reads=[h2Tf.R, rw.R], writes=[lb.R])
                    P.op("dve", lambda e, lb=lb: e.tensor_tensor(out=lg.t[:], in0=lb.t[:, 0:NE], in1=rbb.t[:], op=ALU.add), reads=[lb.R, rbb.R], writes=[lg.R])
                    if self.debug.get("stop") == "E2c":
                        return
                    if NEX < NE:
                        P.op("dve", lambda e: e.memset(lg.t[:, NEX:NE], -1e30), reads=[lg.R], writes=[lg.R])
                    P.op("dve", lambda e: e.max(out=top8.t[:], in_=lg.t[:]), reads=[lg.R], writes=[top8.R])
                    P.op("dve", lambda e: e.tensor_scalar(out=nmx.t[:], in0=top8.t[:, 0:1], scalar1=-1.0, scalar2=None, op0=ALU.mult), reads=[top8.R], writes=[nmx.R])
                    P.op("dve", lambda e: e.tensor_scalar(out=msk.t[:], in0=lg.t[:], scalar1=top8.t[:, 3:4], scalar2=None, op0=ALU.is_ge),
                         reads=[lg.R, top8.R], writes=[msk.R])
                    P.op("act", lambda e: e.activation(out=ex.t[:], in_=lg.t[:], func=AF.Exp, bias=nmx.t[:, 0:1], scale=1.0), reads=[lg.R, nmx.R], writes=[ex.R])
                    P.op("dve", lambda e: e.tensor_tensor(out=ex.t[:], in0=ex.t[:], in1=msk.t[:], op=ALU.mult), reads=[ex.R, msk.R], writes=[ex.R])
                    P.op("dve", lambda e: e.tensor_reduce(out=ssum.t[:], in_=ex.t[:], axis=AX.X, op=ALU.add), reads=[ex.R], writes=[ssum.R])
                    P.op("dve", lambda e: e.reciprocal(out=ssum.t[:], in_=ssum.t[:]), reads=[ssum.R], writes=[ssum.R])
                    P.op("dve", lambda e, tl=tl: e.tensor_scalar(out=gate.t[:, tl, :], in0=ex.t[:], scalar1=ssum.t[:, 0:1], scalar2=None, op0=ALU.mult),
                         reads=[ex.R, ssum.R], writes=[gate.regs[tl]])
                    if self.debug.get("stop") == "E2b":
                        return
                    gb = ps[5]
                    P.op("pe", lambda e, tl=tl, gb=gb: e.transpose(out=gb.t[0:NE, 0:128], in_=gate.t[:, tl, :], identity=C["ident_f"].t[:]),
                         reads=[gate.regs[tl], C["ident_f"].R], writes=[gb.R])
                    P.op("dve", lambda e, tl=tl, gb=gb: e.tensor_copy(out=gTs.t[:, tl * 128:(tl + 1) * 128], in_=gb.t[0:NE, 0:128]), reads=[gb.R], writes=[gTs.R])
                    for ch in range(8):
                        bank = ps[6 + ch % 2]
                        P.op("pe", lambda e, tl=tl, ch=ch, bank=bank: e.matmul(bank.t[:, 0:512], lhsT=gTs.t[:, tl * 128:(tl + 1) * 128],
                                                                               rhs=bdn.t[:, ch * 512:(ch + 1) * 512], start=True, stop=True),
                             reads=[gTs.R, bdn.R], writes=[bank.R])
                        P.op("dve", lambda e, tl=tl, ch=ch, bank=bank: e.tensor_tensor(out=acc.t[:, tl, ch * 512:(ch + 1) * 512],
                                                                                       in0=acc.t[:, tl, ch * 512:(ch + 1) * 512], in1=bank.t[:, 0:512], op=ALU.add),
                             reads=[bank.R, acc.regs[tl]], writes=[acc.regs[tl]])
            if self.debug.get("dump"):
                dg = self.dbg_t("dbg_gate", [T, NE], F32); da = self.dbg_t("dbg_acc0", [T, D], F32); db_ = self.dbg_t("dbg_bupT", [128, 16 * NE], F32)
                for tl in range(NT):
                    tt = half * NT + tl
                    self.load("sp", dg.ap()[tt * 128:(tt + 1) * 128, :], gate.t[:, tl, :], [gate.regs[tl]], [Region()])
                    self.load("sp", da.ap()[tt * 128:(tt + 1) * 128, :], acc.t[:, tl, :], [acc.regs[tl]], [Region()])
                self.load("sp", db_.ap(), bupT.t[:].rearrange("p a b -> p (a b)"), [bupT.R], [Region()])
                P.fence()
            if self.debug.get("stop") == "E2":
                return
            with self.scope() as es2:
                wu = [self.sb(es2, "wu%d" % i, [128, KC, 256], BF16) for i in range(2)]
                wd = [self.sb(es2, "wd%d" % i, [128, 8, 512], BF16) for i in range(2)]
                tg = self.sb(es2, "tg", [128, 8, NTOK], F32, 8)
                aT = [self.sb(es2, "aT%d" % i, [128, 8, NTOK], BF16, 8) for i in range(2)]
                g1 = [self.sb(es2, "g1_%d" % i, [128, NTOK], F32) for i in range(2)]
                s1 = [self.sb(es2, "s1_%d" % i, [128, NTOK], F32) for i in range(2)]
                u1 = [self.sb(es2, "u1_%d" % i, [128, NTOK], F32) for i in range(2)]
                nu = nd = nb = 0
                for ex_ in range(NEX):
                    wup_v = w_up.ap()[ex_].rearrange("(k p) c -> p k c", p=128)
                    wdn_v = w_down.ap()[ex_].rearrange("(k p) c -> p k c", p=128)
                    aTe = aT[ex_ % 2]
                    for cp in range(8):
                        w = wu[nu % 2]
                        nu += 1
                        for a, b in ((0, 16), (16, 32)):
                            self.load("pool", w.t[:, a:b, :], wup_v[:, a:b, cp * 256:(cp + 1) * 256], [], [w.R], max_dma_last_dim=4096)
                        for sub in range(2):
                            c = cp * 2 + sub
                            bank = ps[nb % 4]
                            nb += 1

                            def f_up(e, sub=sub, bank=bank, w=w):
                                for kc in range(KC):
                                    ins = e.matmul(bank.t[:, 0:NTOK], lhsT=w.t[:, kc, sub * 128:(sub + 1) * 128], rhs=h2T.t[:, kc, :],
                                                   start=(kc == 0), stop=(kc == KC - 1))
                                return ins
                            P.op("pe", f_up, reads=[w.R] + h2T.regs, writes=[bank.R])
                            i2 = c % 2
                            bcol = bupT.t[:, c, ex_:ex_ + 1]
                            if c < 8:
                                P.op("dve", lambda e, bank=bank, i2=i2, bcol=bcol: e.tensor_scalar(out=g1[i2].t[:], in0=bank.t[:, 0:NTOK], scalar1=bcol,
                                                                                                    scalar2=7.0, op0=ALU.add, op1=ALU.min),
                                     reads=[bank.R, bupT.R], writes=[g1[i2].R])
                                P.op("act", lambda e, i2=i2: e.activation(out=s1[i2].t[:], in_=g1[i2].t[:], func=AF.Sigmoid, scale=1.702),
                                     reads=[g1[i2].R], writes=[s1[i2].R])
                                P.op("dve", lambda e, i2=i2, c=c: e.tensor_tensor(out=tg.t[:, c, :], in0=g1[i2].t[:], in1=s1[i2].t[:], op=ALU.mult),
                                     reads=[g1[i2].R, s1[i2].R], writes=[tg.regs[c]])
                            else:
                                f = c - 8
                                P.op("dve", lambda e, bank=bank, i2=i2, bcol=bcol: e.tensor_scalar(out=u1[i2].t[:], in0=bank.t[:, 0:NTOK], scalar1=bcol,
                                                                                                    scalar2=7.0, op0=ALU.add, op1=ALU.min),
                                     reads=[bank.R, bupT.R], writes=[u1[i2].R])
                                P.op("dve", lambda e, i2=i2: e.tensor_scalar(out=u1[i2].t[:], in0=u1[i2].t[:], scalar1=-7.0, scalar2=1.0, op0=ALU.max, op1=ALU.add),
                                     reads=[u1[i2].R], writes=[u1[i2].R])
                                P.op("dve", lambda e, i2=i2, f=f, aTe=aTe: e.tensor_tensor(out=aTe.t[:, f, :], in0=u1[i2].t[:], in1=tg.t[:, f, :], op=ALU.mult),
                                     reads=[u1[i2].R, tg.regs[f]], writes=[aTe.regs[f]])
                    if self.debug.get("dump") and ex_ == 0 and half == 0:
                        dT = self.dbg_t("dbg_aT", [128, 8 * NTOK], BF16)
                        self.load("sp", dT.ap(), aTe.t[:].rearrange("p a b -> p (a b)"), list(aTe.regs), [Region()])
                    for ch in range(8):
                        w2 = wd[nd % 2]
                        nd += 1
                        self.load("pool", w2.t[:], wdn_v[:, :, ch * 512:(ch + 1) * 512], [], [w2.R], max_dma_last_dim=4096)
                        for tl in range(NT):
                            bank = ps[4 + nb % 4]
                            nb += 1

                            def f_dn(e, tl=tl, bank=bank, w2=w2, aTe=aTe):
                                for fc in range(8):
                                    ins = e.matmul(bank.t[:, 0:512], lhsT=aTe.t[:, fc, tl * 128:(tl + 1) * 128], rhs=w2.t[:, fc, :], start=(fc == 0), stop=(fc == 7))
                                return ins
                            P.op("pe", f_dn, reads=[w2.R] + aTe.regs, writes=[bank.R])
                            P.op("dve", lambda e, tl=tl, ch=ch, bank=bank, ex_=ex_: e.scalar_tensor_tensor(
                                out=acc.t[:, tl, ch * 512:(ch + 1) * 512], in0=bank.t[:, 0:512], scalar=gate.t[:, tl, ex_:ex_ + 1],
                                in1=acc.t[:, tl, ch * 512:(ch + 1) * 512], op0=ALU.mult, op1=ALU.add),
                                reads=[bank.R, gate.regs[tl], acc.regs[tl]], writes=[acc.regs[tl]])
            if self.debug.get("dump"):
                da1 = self.dbg_t("dbg_acc1", [T, D], F32)
                for tl in range(NT):
                    tt = half * NT + tl
                    self.load("sp", da1.ap()[tt * 128:(tt + 1) * 128, :], acc.t[:, tl, :], [acc.regs[tl]], [Region()])
                P.fence()
            with self.scope() as es2:
                gbc = self.sb(es2, "gbc3", [128, D], F32)
                bbc = self.sb(es2, "bbc3", [128, D], F32)
                self.load("sp", gbc.t[:], ln_moe_g.ap().partition_broadcast(128), [], [gbc.R])
                self.load("sp", bbc.t[:], ln_moe_b.ap().partition_broadcast(128), [], [bbc.R])
                tmp = self.ln_rows(es2, "lnO")
                xo_ = [self.sb(es2, "xout%d" % i, [128, D], F32) for i in range(2)]
                for tl in range(NT):
                    tt = half * NT + tl
                    xfin = xo_[tl % 2]
                    P.op("act", lambda e, tl=tl, xfin=xfin: e.activation(out=xfin.t[:], in_=acc.t[:, tl, :], func=AF.Copy), reads=[acc.regs[tl]], writes=[xfin.R])
                    self.ln_tile(xfin, tmp, LN_EPS, gbc, bbc)
                    self.load("sp", out_t.ap()[tt * 128:(tt + 1) * 128, :], xfin.t[:], [xfin.R], [out_regs[tt]])


def _consts(j):
    bf = ml_dtypes.bfloat16
    idx = np.arange(128)
    c = {}
    c["c_ident_f"] = np.eye(128, dtype=np.float32)
    c["c_ident_b"] = np.eye(128, dtype=np.float32).astype(bf)
    c["c_ones_b"] = np.ones((128, 128), np.float32).astype(bf)
    c["c_ones_f"] = np.ones((128, 128), np.float32)
    c["c_tri_f"] = (idx[:, None] <= idx[None, :]).astype(np.float32)
    c["c_uincl_f"] = (idx[:, None] >= idx[None, :]).astype(np.float32)
    sel = np.zeros((96, NFH * 128), np.float32)
    for h in range(NFH):
        for r in (h, 32 + h, 64 + h):
            sel[r, h * 128:(h + 1) * 128] = 1.0
    c["c_sel96"] = sel.astype(bf)
    c["c_jones"] = np.full((128, 128), float(j), np.float32)
    vis_incl = (idx[:, None] <= idx[None, :]).astype(np.float32)
    vis_strict = (idx[:, None] < idx[None, :]).astype(np.float32)
    allv = np.ones((128, 128), np.float32)
    nonev = np.zeros((128, 128), np.float32)
    fox = [vis_incl, nonev, nonev] if j == 0 else [allv, allv, vis_incl]
    sb = [vis_strict, nonev, nonev] if j == 0 else [allv, allv, vis_strict]
    c["c_mbn"] = np.stack([(1.0 - m) * NEG for m in fox], axis=1).astype(bf)
    c["c_mbp"] = np.stack([(1.0 - m) * (-NEG) for m in sb], axis=1).astype(bf)
    c["c_m01"] = np.stack(sb, axis=1).astype(np.float32)
    return c


def make_in_maps(inputs, names=None, n_cores=N_CORES):
    f = lambda a: np.ascontiguousarray(np.asarray(a, dtype=np.float32))
    shared = {
        "ln_in_g": f(inputs["ln_in_g"]), "ln_in_b": f(inputs["ln_in_b"]),
        "w_in": f(inputs["w_in"][0]), "b_f": f(inputs["b_f"][0]),
        "fox_q_norm_g": f(inputs["fox_q_norm_g"][0]), "fox_k_norm_g": f(inputs["fox_k_norm_g"][0]),
        "mix_norm_g": f(inputs["mix_norm_g"][0]), "w_out": f(inputs["w_out"][0]),
        "ln_mix_g": f(inputs["ln_mix_g"][0]), "ln_mix_b": f(inputs["ln_mix_b"][0]),
        "mem_ln_g": f(inputs["mem_ln_g"][0]), "mem_ln_b": f(inputs["mem_ln_b"][0]),
        "xa_wq": f(inputs["xa_wq"][0]), "xa_wkv": f(inputs["xa_wkv"][0]), "xa_wo": f(inputs["xa_wo"][0]),
        "ln_xa_g": f(inputs["ln_xa_g"][0]), "ln_xa_b": f(inputs["ln_xa_b"][0]),
        "router_w": f(inputs["router_w"][0]), "router_b": f(inputs["router_b"][0]),
        "w_up": f(inputs["w_up"][0]), "b_up": f(inputs["b_up"][0]),
        "w_down": f(inputs["w_down"][0]), "b_down": f(inputs["b_down"][0]),
        "ln_moe_g": f(inputs["ln_moe_g"][0]), "ln_moe_b": f(inputs["ln_moe_b"][0]),
    }
    x = np.asarray(inputs["x"], dtype=np.float32)
    mem = np.asarray(inputs["mem"], dtype=np.float32)
    maps = []
    for c in range(n_cores):
        b, j = c // 2, c % 2
        m = dict(shared)
        m["x_seq"] = np.ascontiguousarray(x[b])
        m["x_own"] = np.ascontiguousarray(x[b, j * T:(j + 1) * T])
        m["mem"] = np.ascontiguousarray(mem[b])
        m.update(_consts(j))
        if names is not None:
            m = {k: v for k, v in m.items() if k in names}
        maps.append(m)
    return maps


_NC_CACHE = {}


def kernel(**inputs):
    if "nc" not in _NC_CACHE:
        bld = Builder()
        _NC_CACHE["nc"] = bld.build()
        _NC_CACHE["names"] = set(bld.inputs.keys())
    nc = _NC_CACHE["nc"]
    maps = make_in_maps(inputs, _NC_CACHE["names"])
    res = run_bass_kernel_spmd(nc, maps, core_ids=list(range(N_CORES)))
    out = np.empty((4, S, D), np.float32)
    for c in range(N_CORES):
        b, j = c // 2, c % 2
        out[b, j * T:(j + 1) * T] = res.results[c]["out"]
    return out
```
